# Optimizing a Trainium2 kernel written in Bass

```python
import jax, jax.numpy as jnp
from jax import lax
import numpy as np

D_MODEL = 1024
BATCH = 16
SEQ = 2048
DEPTH = 1

N_MEM = 256
EPS = 1e-6

HEAD_DIM = 64
DIL_PATTERNS = ((128, 1), (512, 4), (2048, 16))
N_GROUPS = len(DIL_PATTERNS)
ATTN_SLOTS = 4
ATTN_HEADS = N_GROUPS * ATTN_SLOTS
ATTN_WIDTH = ATTN_HEADS * HEAD_DIM
ATTN_OUT = ATTN_SLOTS * HEAD_DIM
ROT_DIM = HEAD_DIM // 4
ROPE_THETA = 500000.0

CONV_WIDTH = 768
CONV_K = 3

MEM_HEADS = 4
MEM_HEAD_DIM = 128
MEM_WIDTH = MEM_HEADS * MEM_HEAD_DIM

N_BRANCH = 3
IN_SIZES = (ATTN_WIDTH, ATTN_WIDTH, ATTN_WIDTH, CONV_WIDTH, CONV_WIDTH, CONV_WIDTH,
            MEM_WIDTH, N_BRANCH * D_MODEL)
IN_COLS = sum(IN_SIZES)
IN_SPLITS = tuple(int(c) for c in np.cumsum(IN_SIZES)[:-1])

N_EXPERT_GROUPS = 4
EXPERTS_PER_GROUP = 8
N_EXPERTS = N_EXPERT_GROUPS * EXPERTS_PER_GROUP
TOP_K = 2
EXPERT_FF = 512
MOE_BLOCK = 256

kernel_name = 'hybrid_dilated_conv_mem_hmoe_block'


def _rmsnorm(x, g):
    xf = x.astype(jnp.float32)
    y = xf * lax.rsqrt(jnp.mean(xf * xf, axis=-1, keepdims=True) + EPS)
    return (y * g.astype(jnp.float32)).astype(x.dtype)


def _rope_tables(positions):
    inv = ROPE_THETA ** (-jnp.arange(0, ROT_DIM, 2, dtype=jnp.float32) / ROT_DIM)
    ang = positions.astype(jnp.float32)[..., None] * inv
    return jnp.cos(ang)[:, :, None, :], jnp.sin(ang)[:, :, None, :]


def _partial_rope(t, cos, sin):
    tr = t[..., :ROT_DIM].astype(jnp.float32)
    t1, t2 = tr[..., :ROT_DIM // 2], tr[..., ROT_DIM // 2:]
    rot = jnp.concatenate([t1 * cos - t2 * sin, t2 * cos + t1 * sin], axis=-1)
    return jnp.concatenate([rot.astype(t.dtype), t[..., ROT_DIM:]], axis=-1)


def _banded_attention(q, k, v, half):
    b, g, L, h, dh = q.shape
    blk = half
    nb = -(-L // blk)
    lp = nb * blk
    qb = jnp.pad(q, ((0, 0), (0, 0), (0, lp - L), (0, 0), (0, 0))).reshape(b, g, nb, blk, h, dh)
    pad_k = ((0, 0), (0, 0), (blk, lp - L + blk), (0, 0), (0, 0))

    def windows(t):
        tb = jnp.pad(t, pad_k).reshape(b, g, nb + 2, blk, h, dh)
        return jnp.concatenate([tb[:, :, :-2], tb[:, :, 1:-1], tb[:, :, 2:]], axis=3)

    kw, vw = windows(k), windows(v)
    qpos = np.arange(nb)[:, None] * blk + np.arange(blk)[None, :]
    kpos = np.arange(nb)[:, None] * blk - blk + np.arange(3 * blk)[None, :]
    valid = ((np.abs(qpos[:, :, None] - kpos[:, None, :]) <= half)
             & (kpos[:, None, :] >= 0) & (kpos[:, None, :] < L))
    s = jnp.einsum('bgnqhd,bgnkhd->bgnhqk', qb, kw).astype(jnp.float32) * (dh ** -0.5)
    s = jnp.where(jnp.asarray(valid)[:, None], s, jnp.finfo(jnp.float32).min)
    m = jnp.max(s, axis=-1, keepdims=True)
    p = jnp.exp(s - m)
    den = jnp.sum(p, axis=-1)
    o = jnp.einsum('bgnhqk,bgnkhd->bgnqhd', p.astype(v.dtype), vw)
    o = o / jnp.transpose(den, (0, 1, 2, 4, 3))[..., None].astype(o.dtype)
    lse = jnp.transpose(m[..., 0] + jnp.log(den), (0, 1, 2, 4, 3))
    o = o.reshape(b, g, lp, h, dh)[:, :, :L]
    lse = lse.reshape(b, g, lp, h)[:, :, :L]
    return o, lse


def _dilated_attention(q, k, v, window, dilation):
    b, s, h, dh = q.shape
    L = s // dilation
    half = window // (2 * dilation)

    def to_sub(t):
        return jnp.transpose(t.reshape(b, L, dilation, h, dh), (0, 2, 1, 3, 4))

    o, lse = _banded_attention(to_sub(q), to_sub(k), to_sub(v), half)
    o = jnp.transpose(o, (0, 2, 1, 3, 4)).reshape(b, s, h, dh)
    lse = jnp.transpose(lse, (0, 2, 1, 3)).reshape(b, s, h)
    return o, lse


def _hierarchical_moe(h, w_rg, b_rg, w_re, b_re, w_gate, w_up, w_down):
    b, s, d = h.shape
    n = b * s
    hf = h.reshape(n, d)
    hr = hf.astype(jnp.float32)
    pg = jax.nn.softmax(hr @ w_rg.astype(jnp.float32) + b_rg.astype(jnp.float32), axis=-1)
    pg_top, g_idx = lax.top_k(pg, 1)
    el = (hr @ w_re.astype(jnp.float32) + b_re.astype(jnp.float32)).reshape(n, N_EXPERT_GROUPS, EXPERTS_PER_GROUP)
    sel = jnp.einsum('ng,nge->ne', jax.nn.one_hot(g_idx[:, 0], N_EXPERT_GROUPS, dtype=jnp.float32), el)
    pe_top, e_idx = lax.top_k(jax.nn.softmax(sel, axis=-1), TOP_K)
    weights = pg_top * pe_top / jnp.sum(pe_top, axis=-1, keepdims=True)
    ids = g_idx * EXPERTS_PER_GROUP + e_idx

    nk = n * TOP_K
    e = ids.reshape(nk).astype(jnp.int32)
    w = weights.reshape(nk)
    t = jnp.repeat(jnp.arange(n, dtype=jnp.int32), TOP_K)
    order = jnp.argsort(e, stable=True)
    e_s, t_s, w_s = e[order], t[order], w[order]
    counts = jnp.bincount(e, length=N_EXPERTS)
    padded = ((counts + MOE_BLOCK - 1) // MOE_BLOCK) * MOE_BLOCK
    start = jnp.cumsum(counts) - counts
    pend = jnp.cumsum(padded)
    pstart = pend - padded
    dest = pstart[e_s] + (jnp.arange(nk, dtype=jnp.int32) - start[e_s])
    n_rows = (-(-nk // MOE_BLOCK) + N_EXPERTS) * MOE_BLOCK
    n_blk = n_rows // MOE_BLOCK
    row_tok = jnp.zeros((n_rows,), jnp.int32).at[dest].set(t_s)
    row_w = jnp.zeros((n_rows,), jnp.float32).at[dest].set(w_s)
    blk_exp = jnp.minimum(jnp.searchsorted(pend, jnp.arange(n_blk) * MOE_BLOCK, side='right'),
                          N_EXPERTS - 1).astype(jnp.int32)

    def expert_block(args):
        tok, wt, ex = args
        xb = hf[tok]
        a = xb @ w_gate[ex]
        u = xb @ w_up[ex]
        y = (jax.nn.silu(a) * u) @ w_down[ex]
        return y * wt[:, None].astype(y.dtype)

    ys = lax.map(expert_block, (row_tok.reshape(n_blk, MOE_BLOCK),
                                row_w.reshape(n_blk, MOE_BLOCK), blk_exp))
    out = jnp.zeros((n, d), h.dtype).at[row_tok].add(ys.reshape(n_rows, d).astype(h.dtype))
    return out.reshape(b, s, d)


def setup_inputs(seed: int = 0) -> dict:
    key = jax.random.key(seed)
    ks = jax.random.split(key, 24)
    f32 = jnp.float32

    def nrm(k, shape, scale):
        return jax.random.normal(k, shape, f32) * scale

    def gain(k, dim):
        return 1.0 + 0.05 * jax.random.normal(k, (DEPTH, dim), f32)

    x = jax.random.normal(ks[0], (BATCH, SEQ, D_MODEL), f32)
    mem = jax.random.normal(ks[1], (BATCH, N_MEM, D_MODEL), f32)
    positions = (jnp.arange(SEQ, dtype=jnp.int32)[None, :]
                 + jax.random.randint(ks[2], (BATCH, 1), 0, 4096, dtype=jnp.int32))
    return {
        'x': x,
        'mem': mem,
        'positions': positions,
        'g_mix': gain(ks[3], D_MODEL),
        'g_mem': gain(ks[4], D_MODEL),
        'w_in': nrm(ks[5], (DEPTH, D_MODEL, IN_COLS), D_MODEL ** -0.5),
        'g_qn_attn': gain(ks[6], HEAD_DIM),
        'g_kn_attn': gain(ks[7], HEAD_DIM),
        'w_conv': nrm(ks[8], (DEPTH, CONV_K, CONV_WIDTH), CONV_K ** -0.5),
        'w_mem_kv': nrm(ks[9], (DEPTH, D_MODEL, 2 * MEM_WIDTH), D_MODEL ** -0.5),
        'g_qn_mem': gain(ks[10], MEM_HEAD_DIM),
        'g_kn_mem': gain(ks[11], MEM_HEAD_DIM),
        'w_proj_attn': nrm(ks[12], (DEPTH, ATTN_OUT, D_MODEL), ATTN_OUT ** -0.5),
        'w_proj_conv': nrm(ks[13], (DEPTH, CONV_WIDTH, D_MODEL), CONV_WIDTH ** -0.5),
        'w_proj_mem': nrm(ks[14], (DEPTH, MEM_WIDTH, D_MODEL), MEM_WIDTH ** -0.5),
        'w_out': nrm(ks[15], (DEPTH, D_MODEL, D_MODEL), D_MODEL ** -0.5),
        'g_ffn': gain(ks[16], D_MODEL),
        'w_router_group': nrm(ks[17], (DEPTH, D_MODEL, N_EXPERT_GROUPS), D_MODEL ** -0.5),
        'b_router_group': nrm(ks[18], (DEPTH, N_EXPERT_GROUPS), 0.01),
        'w_router_expert': nrm(ks[19], (DEPTH, D_MODEL, N_EXPERTS), D_MODEL ** -0.5),
        'b_router_expert': nrm(ks[20], (DEPTH, N_EXPERTS), 0.01),
        'w_gate': nrm(ks[21], (DEPTH, N_EXPERTS, D_MODEL, EXPERT_FF), D_MODEL ** -0.5),
        'w_up': nrm(ks[22], (DEPTH, N_EXPERTS, D_MODEL, EXPERT_FF), D_MODEL ** -0.5),
        'w_down': nrm(ks[23], (DEPTH, N_EXPERTS, EXPERT_FF, D_MODEL), EXPERT_FF ** -0.5),
    }


def reference(x, mem, positions, g_mix, g_mem, w_in, g_qn_attn, g_kn_attn, w_conv,
              w_mem_kv, g_qn_mem, g_kn_mem, w_proj_attn, w_proj_conv, w_proj_mem, w_out,
              g_ffn, w_router_group, b_router_group, w_router_expert, b_router_expert,
              w_gate, w_up, w_down):
    b, s, d = x.shape
    cos, sin = _rope_tables(positions)
    cos, sin = cos.astype(x.dtype), sin.astype(x.dtype)
    for l in range(DEPTH):
        h = _rmsnorm(x, g_mix[l])
        proj = h @ w_in[l]
        q_a, k_a, v_a, cx, cb, cc, q_m, gate_logits = jnp.split(proj, IN_SPLITS, axis=-1)

        q_a = _partial_rope(_rmsnorm(q_a.reshape(b, s, ATTN_HEADS, HEAD_DIM), g_qn_attn[l]), cos, sin)
        k_a = _partial_rope(_rmsnorm(k_a.reshape(b, s, ATTN_HEADS, HEAD_DIM), g_kn_attn[l]), cos, sin)
        v_a = v_a.reshape(b, s, ATTN_HEADS, HEAD_DIM)
        q_a = q_a.reshape(b, s, N_GROUPS, ATTN_SLOTS, HEAD_DIM)
        k_a = k_a.reshape(b, s, N_GROUPS, ATTN_SLOTS, HEAD_DIM)
        v_a = v_a.reshape(b, s, N_GROUPS, ATTN_SLOTS, HEAD_DIM)
        outs, lses = [], []
        for gi, (window, dilation) in enumerate(DIL_PATTERNS):
            o_g, lse_g = _dilated_attention(q_a[:, :, gi], k_a[:, :, gi], v_a[:, :, gi], window, dilation)
            outs.append(o_g)
            lses.append(lse_g)
        alpha = jax.nn.softmax(jnp.stack(lses, axis=2), axis=2)
        o_attn = jnp.einsum('bsgh,bsghd->bshd', alpha.astype(x.dtype),
                            jnp.stack(outs, axis=2)).reshape(b, s, ATTN_OUT)

        u = cc * cx
        up = jnp.pad(u, ((0, 0), (1, 1), (0, 0)))
        wc = w_conv[l]
        y_conv = wc[0] * up[:, :-2] + wc[1] * up[:, 1:-1] + wc[2] * up[:, 2:]
        z_conv = cb * y_conv

        mkv = _rmsnorm(mem, g_mem[l]) @ w_mem_kv[l]
        k_m, v_m = jnp.split(mkv, 2, axis=-1)
        k_m = _rmsnorm(k_m.reshape(b, N_MEM, MEM_HEADS, MEM_HEAD_DIM), g_kn_mem[l])
        v_m = v_m.reshape(b, N_MEM, MEM_HEADS, MEM_HEAD_DIM)
        q_m = _rmsnorm(q_m.reshape(b, s, MEM_HEADS, MEM_HEAD_DIM), g_qn_mem[l])
        sm = jnp.einsum('bshd,bmhd->bhsm', q_m, k_m).astype(jnp.float32) * (MEM_HEAD_DIM ** -0.5)
        pm = jax.nn.softmax(sm, axis=-1).astype(v_m.dtype)
        o_mem = jnp.einsum('bhsm,bmhd->bshd', pm, v_m).reshape(b, s, MEM_WIDTH)

        gates = jax.nn.sigmoid(gate_logits).reshape(b, s, N_BRANCH, d)
        merged = (gates[:, :, 0] * (o_attn @ w_proj_attn[l])
                  + gates[:, :, 1] * (z_conv @ w_proj_conv[l])
                  + gates[:, :, 2] * (o_mem @ w_proj_mem[l]))
        x = x + merged @ w_out[l]

        h2 = _rmsnorm(x, g_ffn[l])
        x = x + _hierarchical_moe(h2, w_router_group[l], b_router_group[l], w_router_expert[l],
                                  b_router_expert[l], w_gate[l], w_up[l], w_down[l])
    return x
```

```python
import numpy as np
import concourse.bass as bass
import concourse.mybir as mybir
from concourse.bass_utils import run_bass_kernel_spmd

F32 = mybir.dt.float32
BF16 = mybir.dt.bfloat16
I32 = mybir.dt.int32
ALU = mybir.AluOpType
AF = mybir.ActivationFunctionType
AX = mybir.AxisListType

NCORES = 8
SEQ = 2048
D = 1024
NSEQ = 2
NTOK = NSEQ * SEQ
NT = SEQ // 128
DIL = (1, 4, 16)
EPS = 1e-6
NEXP = 32
CAP = 384
NROWS = NEXP * CAP
DEBUG = False
STOP = None
ATT = 0


class Sched:
    ENG = ("pe", "act", "dve", "pool", "sp")

    def __init__(self, nc):
        self.nc = nc
        self.ops = {e: [] for e in self.ENG}
        self.res = {}
        self.dma_keys = {}
        self.all_ops = []

    def _r(self, key):
        r = self.res.get(key)
        if r is None:
            r = self.res[key] = [None, []]
        return r

    def add(self, eng, fn, reads=(), writes=(), dma_key=None, extra_deps=()):
        op = dict(eng=eng, fn=fn, dma_key=dma_key, signal=False, cnt=0)
        deps = list(extra_deps)
        for k in reads:
            r = self._r(k)
            if r[0] is not None:
                deps.append(r[0])
        for k in writes:
            r = self._r(k)
            if r[0] is not None:
                deps.append(r[0])
            deps.extend(r[1])
        for k in reads:
            self._r(k)[1].append(op)
        for k in writes:
            r = self._r(k)
            r[0] = op
            r[1] = []
        op["deps"] = deps
        if dma_key is not None:
            lst = self.dma_keys.setdefault(dma_key, [])
            lst.append(op)
            op["cnt"] = 16 * len(lst)
        self.ops[eng].append(op)
        self.all_ops.append(op)
        return op

    def barrier(self, dummy):
        last = []
        for e in self.ENG:
            for op in reversed(self.ops[e]):
                if op["dma_key"] is None and op["fn"] is not None:
                    last.append(op)
                    break
        for k, lst in self.dma_keys.items():
            last.append(lst[-1])
        self.res = {}
        for e in self.ENG:
            self.add(e, None, extra_deps=last)

    def emit(self):
        nc = self.nc
        for op in self.all_ops:
            for d in op["deps"]:
                if d["dma_key"] is None and not (d["eng"] == op["eng"] == "pe"):
                    d["signal"] = True
        esem = {e: nc.alloc_semaphore("sem_" + e) for e in self.ENG}
        dsem = {k: nc.alloc_semaphore("dsem_%d" % i) for i, k in enumerate(self.dma_keys)}
        for e in self.ENG:
            c = 0
            for op in self.ops[e]:
                if op["dma_key"] is None and op["signal"]:
                    assert op["fn"] is not None
                    c += 1
                    op["cnt"] = c
        with nc.Block() as block:
            def run(ename, eng):
                waited = {}
                for op in self.ops[ename]:
                    need = {}
                    for d in op["deps"]:
                        if d["dma_key"] is not None:
                            s = dsem[d["dma_key"]]
                        else:
                            if d["eng"] == ename == "pe":
                                continue
                            s = esem[d["eng"]]
                        if d["cnt"] > need.get(s, 0):
                            need[s] = d["cnt"]
                    for s, v in need.items():
                        if waited.get(s, 0) < v:
                            eng.wait_ge(s, v)
                            waited[s] = v
                    if op["fn"] is None:
                        continue
                    ins = op["fn"](eng)
                    if op["dma_key"] is not None:
                        ins.then_inc(dsem[op["dma_key"]], 16)
                    elif op["signal"]:
                        ins.then_inc(esem[ename], 1)
                if ename == "sp":
                    for k, lst in self.dma_keys.items():
                        eng.wait_ge(dsem[k], 16 * len(lst))

            @block.tensor
            def _(eng):
                run("pe", eng)

            @block.scalar
            def _(eng):
                run("act", eng)

            @block.vector
            def _(eng):
                run("dve", eng)

            @block.gpsimd
            def _(eng):
                run("pool", eng)

            @block.sync
            def _(eng):
                run("sp", eng)


def _bytes(shape, dt):
    n = 1
    for s in shape[1:]:
        n *= s
    sz = {F32: 4, BF16: 2, I32: 4}[dt]
    return (n * sz + 31) // 32 * 32


class Arena:
    def __init__(self, nc, base, limit):
        self.nc, self.base, self.limit, self.off = nc, base, limit, base
        self.n = 0

    def alloc(self, name, shape, dt):
        b = _bytes(shape, dt)
        assert self.off + b <= self.limit, (name, self.off, b, self.limit)
        t = self.nc.alloc_sbuf_tensor_at("%s_%d" % (name, Arena.uid()), list(shape), dt, offset=self.off)
        self.off += b
        return t

    _uid = [0]

    @staticmethod
    def uid():
        Arena._uid[0] += 1
        return Arena._uid[0]

    def reset(self):
        self.off = self.base


class Pipe:
    def __init__(self, depth=1):
        self.q, self.depth = [], depth

    def push(self, fn):
        self.q.append(fn)
        while len(self.q) > self.depth:
            self.q.pop(0)()

    def flush(self):
        while self.q:
            self.q.pop(0)()


def build_nc():
    nc = bass.Bass("TRN2", target_bir_lowering=False)
    S = Sched(nc)

    def din(name, shape, dt=F32):
        return nc.dram_tensor(name, list(shape), dt, kind="ExternalInput").ap()

    X = din("x", [NSEQ, SEQ, D])
    MEM = din("mem", [NSEQ, 256, D])
    POS = din("positions", [NSEQ, SEQ], I32)
    G_MIX = din("g_mix", [1, D])
    G_MEM = din("g_mem", [1, D])
    W_IN = din("w_in", [1, D, 8192])
    G_QA = din("g_qn_attn", [1, 64])
    G_KA = din("g_kn_attn", [1, 64])
    W_CONV = din("w_conv", [1, 3, 768])
    W_MKV = din("w_mem_kv", [1, D, 1024])
    G_QM = din("g_qn_mem", [1, 128])
    G_KM = din("g_kn_mem", [1, 128])
    W_PA = din("w_proj_attn", [1, 256, D])
    W_PC = din("w_proj_conv", [1, 768, D])
    W_PM = din("w_proj_mem", [1, 512, D])
    W_OUT = din("w_out", [1, D, D])
    G_FFN = din("g_ffn", [1, D])
    W_RG = din("w_router_group", [1, D, 4])
    B_RG = din("b_router_group", [1, 4])
    W_RE = din("w_router_expert", [1, D, 32])
    B_RE = din("b_router_expert", [1, 32])
    nexp_decl = 1 if (STOP is not None and STOP != "moe") else NEXP
    W_G = din("w_gate", [1, nexp_decl, D, 512])
    W_U = din("w_up", [1, nexp_decl, D, 512])
    W_D = din("w_down", [1, nexp_decl, 512, D])
    OUT = nc.dram_tensor("out", [NTOK, D], F32, kind="ExternalOutput").ap()
    skind = "ExternalOutput" if DEBUG else "Internal"
    X1S = nc.dram_tensor("x1s", [NTOK, D], F32, kind=skind).ap()
    H2S = nc.dram_tensor("h2s", [NTOK, D], BF16, kind=skind).ap()
    ROWTOK = nc.dram_tensor("rowtok", [NROWS + 1024, 1], I32, kind=skind).ap()
    YS = nc.dram_tensor("ys", [NROWS, D], F32, kind=skind).ap()
    if DEBUG:
        DBG_OA = nc.dram_tensor("dbg_oa", [NSEQ, 64, 4 * SEQ], BF16, kind="ExternalOutput").ap()
        DBG_Z = nc.dram_tensor("dbg_z", [NSEQ, 128, 6 * SEQ], BF16, kind="ExternalOutput").ap()
        DBG_OM = nc.dram_tensor("dbg_om", [NSEQ, 128, 4 * SEQ], BF16, kind="ExternalOutput").ap()
        DBG_M = nc.dram_tensor("dbg_m", [NSEQ, 128, 8 * SEQ], BF16, kind="ExternalOutput").ap()
        DBG_H = nc.dram_tensor("dbg_h", [NSEQ, 128, 8 * SEQ], BF16, kind="ExternalOutput").ap()

    WINv = W_IN[0].rearrange("(k p) n -> p k n", p=128)

    def mm(out, lhsT, rhs, start, stop, r, w, skip=False):
        if skip:
            S.add("pe", lambda e: e.matmul(out, lhsT=lhsT, rhs=rhs, start=start, stop=stop,
                                           skip_group_check=True), reads=r, writes=w)
        else:
            S.add("pe", lambda e: e.matmul(out, lhsT=lhsT, rhs=rhs, start=start, stop=stop), reads=r, writes=w)

    def tr(out, in_, ident, r, w):
        S.add("pe", lambda e: e.transpose(out=out, in_=in_, identity=ident), reads=r, writes=w)

    def act(out, in_, func, r, w, scale=None, bias=None, accum=None):
        def f(e):
            kw = {}
            if scale is not None:
                kw["scale"] = scale
            if bias is not None:
                kw["bias"] = bias
            if accum is not None:
                kw["accum_out"] = accum
            return e.activation(out=out, in_=in_, func=func, **kw)
        S.add("act", f, reads=r, writes=w)

    def tt(eng, out, in0, in1, op, r, w):
        S.add(eng, lambda e: e.tensor_tensor(out=out, in0=in0, in1=in1, op=op), reads=r, writes=w)

    def ts(eng, out, in0, s1, s2, op0, op1, r, w):
        if op1 is None:
            S.add(eng, lambda e: e.tensor_scalar(out=out, in0=in0, scalar1=s1, scalar2=None, op0=op0),
                  reads=r, writes=w)
        else:
            S.add(eng, lambda e: e.tensor_scalar(out=out, in0=in0, scalar1=s1, scalar2=s2, op0=op0, op1=op1),
                  reads=r, writes=w)

    def stt(eng, out, in0, scalar, in1, op0, op1, r, w):
        S.add(eng, lambda e: e.scalar_tensor_tensor(out=out, in0=in0, scalar=scalar, in1=in1, op0=op0, op1=op1),
              reads=r, writes=w)

    def cp(eng, out, in_, r, w):
        if eng == "act":
            S.add("act", lambda e: e.activation(out=out, in_=in_, func=AF.Copy), reads=r, writes=w)
        else:
            S.add(eng, lambda e: e.tensor_copy(out=out, in_=in_), reads=r, writes=w)

    def red(out, in_, op, r, w):
        S.add("dve", lambda e: e.tensor_reduce(out=out, in_=in_, axis=AX.X, op=op), reads=r, writes=w)

    def recip(out, in_, r, w):
        S.add("dve", lambda e: e.reciprocal(out=out, in_=in_), reads=r, writes=w)

    def mset(eng, ap, val, w):
        S.add(eng, lambda e: e.memset(ap, val), writes=w)

    def dma(eng, out, in_, r, w, key, slow=False):
        if slow:
            S.add(eng, lambda e: e.dma_start(out=out, in_=in_, allow_slow_non_contiguous=True),
                  reads=r, writes=w, dma_key=key)
        else:
            S.add(eng, lambda e: e.dma_start(out=out, in_=in_), reads=r, writes=w, dma_key=key)

    def gather(out, src, idx, r, w, key, nrows):
        S.add("pool", lambda e: e.indirect_dma_start(
            out=out, out_offset=None, in_=src,
            in_offset=bass.IndirectOffsetOnAxis(ap=idx, axis=0)), reads=r, writes=w, dma_key=key)

    def scatter(dst, idx, src, r, w, key):
        S.add("pool", lambda e: e.indirect_dma_start(
            out=dst, out_offset=bass.IndirectOffsetOnAxis(ap=idx, axis=0), in_=src, in_offset=None),
            reads=r, writes=w, dma_key=key)

    def rsqrt_mean(out, ssq, n, r, w):
        act(out, ssq, AF.Sqrt, list(r) + ["eps_c"], w, scale=1.0 / n, bias=eps_c[:, 0:1])
        recip(out, out, list(w), w)

    PF = [nc.alloc_psum_tensor("pf%d" % i, [128, 512], F32) for i in range(6)]
    PB = [nc.alloc_psum_tensor("pb%d" % i, [128, 8, 128], BF16) for i in range(2)]
    PBF = [PB[i][:].rearrange("p k q -> p (k q)").bitcast(F32) for i in range(2)]
    rot = {"f": 0, "b": 0}

    def pf(lo=0, hi=6):
        i = lo + rot["f"] % (hi - lo)
        rot["f"] += 1
        return PF[i], ("pf", i)

    def pb():
        i = rot["b"] % 2
        rot["b"] += 1
        return PB[i], ("pb", i)

    KB = 1024
    BASE = 17 * KB
    CA = Arena(nc, BASE, BASE + 32 * KB)
    ident = CA.alloc("ident", [128, 128], BF16)
    identf = CA.alloc("identf", [128, 128], F32)
    tmpf = CA.alloc("tmpf", [128, 256], F32)
    maskG = CA.alloc("maskG", [128, 256], BF16)
    mask0 = CA.alloc("mask0", [128, 128], BF16)
    maskD = CA.alloc("maskD", [128, 128], BF16)
    utri = CA.alloc("utri", [128, 128], BF16)
    ones_b = CA.alloc("ones_b", [128, 128], BF16)
    gmixB = CA.alloc("gmixB", [128, D], F32)
    gffnB = CA.alloc("gffnB", [128, D], F32)
    gmemB = CA.alloc("gmemB", [128, D], F32)
    gqkB = CA.alloc("gqkB", [128, 8, 64], F32)
    gqmB = CA.alloc("gqmB", [128, 128], F32)
    gkmB = CA.alloc("gkmB", [128, 128], F32)
    wc18 = CA.alloc("wc18", [18, 128], F32)
    wcol = CA.alloc("wcol", [128, 18], F32)
    invf = CA.alloc("invf", [128, 8], F32)
    posi = CA.alloc("posi", [128, 3, 16], I32)
    posf = CA.alloc("posf", [128, 3, 16], F32)
    ang = CA.alloc("ang", [128, 48, 8], F32)
    cosT = CA.alloc("cosT", [128, 48, 8], F32)
    sinT = CA.alloc("sinT", [128, 48, 8], F32)
    angk = CA.alloc("angk", [128, 48, 8], F32)
    angi = CA.alloc("angi", [128, 48, 8], I32)
    wr = CA.alloc("wr", [128, 8, 36], F32)
    biasB = CA.alloc("biasB", [128, 36], F32)
    ecap = CA.alloc("ecap", [128, 32], F32)
    tok = CA.alloc("tok", [128, 32], I32)
    dest = CA.alloc("dest", [128, 32, 2], I32)
    wgt = CA.alloc("wgt", [128, 32, 2], F32)
    srun = CA.alloc("srun", [128, 32], F32)
    srunb = CA.alloc("srunb", [128, 32], BF16)
    stat = CA.alloc("stat", [128, 64], F32)
    dummy = CA.alloc("dummy", [128, 8], F32)
    zero_i = CA.alloc("zero_i", [128, NROWS // 128], I32)
    pi_c = CA.alloc("pi_c", [128, 1], F32)
    eps_c = CA.alloc("eps_c", [128, 1], F32)
    gcol = CA.alloc("gcol", [128, 4], F32)
    XT_OFF = BASE + 32 * KB
    HT_OFF = BASE + 52 * KB
    R_OFF = BASE + 84 * KB
    R_END = BASE + 206 * KB
    xa = Arena(nc, XT_OFF, HT_OFF)
    xt = [xa.alloc("xt", [128, D], F32) for _ in range(4)]
    hb = [xa.alloc("hb", [128, D], BF16) for _ in range(2)]
    hT = nc.alloc_sbuf_tensor_at("hT", [128, 8, SEQ], BF16, offset=HT_OFF)
    oaT = nc.alloc_sbuf_tensor_at("oaT", [64, 4, SEQ], BF16, offset=R_OFF + 106 * KB)
    omT = nc.alloc_sbuf_tensor_at("omT", [128, 4, SEQ], BF16, offset=R_OFF + 88 * KB)
    zT = nc.alloc_sbuf_tensor_at("zT", [128, 6, SEQ], BF16, offset=R_OFF + 64 * KB)
    mT = nc.alloc_sbuf_tensor_at("mT", [128, 8, SEQ], BF16, offset=R_OFF + 32 * KB)

    def bcast_load(dst, src_row, n, key):
        dma("sp", dst, src_row.partition_broadcast(128), [], [key], key)

    bcast_load(gmixB[:], G_MIX[0:1, :], D, "gmixB")
    bcast_load(gffnB[:], G_FFN[0:1, :], D, "gffnB")
    bcast_load(gmemB[:], G_MEM[0:1, :], D, "gmemB")
    for hh in range(4):
        bcast_load(gqkB[:, hh, :], G_QA[0:1, :], 64, ("gqk", hh))
        bcast_load(gqkB[:, 4 + hh, :], G_KA[0:1, :], 64, ("gqk", 4 + hh))
    bcast_load(gqmB[:], G_QM[0:1, :], 128, "gqmB")
    bcast_load(gkmB[:], G_KM[0:1, :], 128, "gkmB")
    bcast_load(biasB[:, 0:4], B_RG[0:1, :], 4, "biasg")
    bcast_load(biasB[:, 4:36], B_RE[0:1, :], 32, "biase")
    GQK = [("gqk", i) for i in range(8)]
    for hh in range(2):
        dma("sp", gcol[hh * 64:(hh + 1) * 64, 0:1], G_QA.rearrange("o n -> n o"), [], [("gcq", hh)], ("gcq", hh), slow=True)
        dma("sp", gcol[hh * 64:(hh + 1) * 64, 1:2], G_KA.rearrange("o n -> n o"), [], [("gck", hh)], ("gck", hh), slow=True)
        mset("dve", gcol[hh * 64:hh * 64 + 16, 0:2], 1.0, [("gcq", hh), ("gck", hh)])
    dma("sp", gcol[:, 2:3], G_QM.rearrange("o n -> n o"), [], ["gcqm"], "gcqm", slow=True)
    dma("sp", gcol[:, 3:4], G_KM.rearrange("o n -> n o"), [], ["gckm"], "gckm", slow=True)
    dma("sp", wc18[:], W_CONV[0].rearrange("t (c p) -> (t c) p", p=128), [], ["wc18"], "wc18")
    dma("sp", wr[:, :, 0:4], W_RG[0].rearrange("(k p) n -> p k n", p=128), [], ["wrg"], "wrg")
    dma("sp", wr[:, :, 4:36], W_RE[0].rearrange("(k p) n -> p k n", p=128), [], ["wre"], "wre")

    S.add("pool", lambda e: e.memset(identf[:], 0.0), writes=["identf"])
    S.add("pool", lambda e: e.affine_select(out=identf[:], in_=identf[:], pattern=[[-1, 128]],
                                            compare_op=ALU.not_equal, fill=1.0, base=0, channel_multiplier=1),
          reads=["identf"], writes=["identf"])
    cp("dve", ident[:], identf[:], ["identf"], ["ident"])

    def build_mask(dst, rows, cols, conds):
        S.add("pool", lambda e: e.memset(tmpf[:, 0:cols], 1.0), writes=["tmpf"])
        for (base, cm, step) in conds:
            S.add("pool", lambda e, base=base, cm=cm, step=step: e.affine_select(
                out=tmpf[:, 0:cols], in_=tmpf[:, 0:cols], pattern=[[step, cols]],
                compare_op=ALU.is_ge, fill=0.0, base=base, channel_multiplier=cm),
                reads=["tmpf"], writes=["tmpf"])
        cp("dve", dst, tmpf[:, 0:cols], ["tmpf"], ["masks"])

    build_mask(maskG[:], 128, 256, [(0, -1, 1), (128, 1, -1)])
    build_mask(mask0[:], 128, 128, [(64, 1, -1)])
    build_mask(maskD[:], 128, 128, [(64, -1, 1), (64, 1, -1)])
    build_mask(utri[:], 128, 128, [(-1, -1, 1)])
    mset("dve", ones_b[:], 1.0, ["ones_b"])
    mset("dve", pi_c[:], float(np.pi), ["pi_c"])
    mset("dve", eps_c[:], EPS, ["eps_c"])
    mset("dve", srun[:], 0.0, ["srun"])
    mset("dve", srunb[:], 0.0, ["srunb"])
    mset("dve", zero_i[:], 0, ["zero_i"])
    for j in range(8):
        v = float(np.float32(500000.0) ** np.float32(-j / 8.0))
        mset("dve", invf[:, j:j + 1], v, ["invf"])
    S.add("pool", lambda e: e.iota(tok[:], [[128, 32]], base=0, channel_multiplier=1), writes=["tok"])
    S.add("pool", lambda e: e.iota(ecap[:], [[CAP, 32]], base=0, channel_multiplier=0,
                                   allow_small_or_imprecise_dtypes=True), writes=["ecap"])
    pft, kft = pf()
    tr(pft[:, 0:18], wc18[:], identf[0:18, 0:18], ["wc18", "identf"], [kft])
    cp("dve", wcol[:], pft[:, 0:18], [kft], ["wcol"])
    dma("sp", ROWTOK[0:NROWS, :].rearrange("(p j) o -> p (j o)", p=128), zero_i[:], ["zero_i"], ["rowtok0"], "rowtok0")

    S.barrier(dummy)
    if STOP == "const":
        S.emit()
        return nc

    def wload(dst, src, wkey, dkey):
        dma("pool", dst, src, [], [wkey], dkey)

    for b in range(NSEQ):
        pv0 = POS[b].rearrange("(t p) -> p t", p=128)
        pv1 = POS[b].rearrange("(t p r) -> p r t", t=4, p=128, r=4)
        pv2 = POS[b].rearrange("(p r) -> p r", r=16)
        dma("sp", posi[:, 0, :], pv0, [], [("posi", 0)], ("posi", 0), slow=True)
        dma("sp", posi[:, 1, :].rearrange("p (r t) -> p r t", r=4), pv1, [], [("posi", 1)], ("posi", 1), slow=True)
        dma("sp", posi[:, 2, :], pv2, [], [("posi", 2)], ("posi", 2), slow=True)
        PK = [("posi", i) for i in range(3)]
        cp("dve", posf[:], posi[:], PK, ["posf"])
        pfl = posf[:].rearrange("p g t -> p (g t)")
        tt("dve", ang[:], pfl.unsqueeze(2).to_broadcast([128, 48, 8]),
           invf[:].unsqueeze(1).to_broadcast([128, 48, 8]), ALU.mult, ["posf", "invf"], ["ang"])
        def sin_of(dst, shift):
            ts("dve", angk[:], ang[:], shift, 1.0 / (2 * np.pi), ALU.add, ALU.mult, ["ang"], ["angk"])
            cp("dve", angi[:], angk[:], ["angk"], ["angi"])
            cp("dve", angk[:], angi[:], ["angi"], ["angk"])
            ts("dve", dst, ang[:], shift, None, ALU.add, None, ["ang"], ["sdst"])
            stt("dve", dst, angk[:], -2 * np.pi, dst, ALU.mult, ALU.add, ["angk", "sdst"], ["sdst"])
            ts("dve", angk[:], dst, float(np.pi), None, ALU.is_gt, None, ["sdst"], ["angk"])
            stt("dve", dst, angk[:], -2 * np.pi, dst, ALU.mult, ALU.add, ["angk", "sdst"], ["sdst"])
            ts("dve", dst, dst, -float(np.pi), float(np.pi), ALU.max, ALU.min, ["sdst"], ["sdst"])
            act(dst, dst, AF.Sin, ["sdst"], ["sdst"])

        sin_of(sinT[:], 0.0)
        sin_of(cosT[:], float(np.pi / 2))
        def norm_rows(src_ap, gB, gkey, dstT, dcol0, xs, hs, statcol, dkey):
            dma("sp", xt[xs][:], src_ap, [], [("xt", xs)], ("xt", xs))
            sq = stat[:, statcol:statcol + 1]
            act(hb[hs][:], xt[xs][:], AF.Square, [("xt", xs)], [("hb", hs), ("stat", statcol)], accum=sq)
            rsqrt_mean(sq, sq, D, [("stat", statcol)], [("stat", statcol)])
            stt("dve", hb[hs][:], xt[xs][:], sq, gB[:], ALU.mult, ALU.mult,
                [("xt", xs), ("stat", statcol), gkey], [("hb", hs)])
            pbt, kb = pb()
            for k in range(8):
                tr(pbt[:, k, :], hb[hs][:, k * 128:(k + 1) * 128], ident[:], [("hb", hs), "ident"], [kb])
            npipe.push(lambda pbt=pbt, kb=kb: cp("dve", dstT[:, :, dcol0:dcol0 + 128], pbt[:], [kb], [dkey]))

        npipe = Pipe(1)
        for t in range(NT):
            norm_rows(X[b, t * 128:(t + 1) * 128, :], gmixB, "gmixB", hT, t * 128, t % 4, t % 2, t % 4, ("hT", t))
        npipe.flush()
        HTK = [("hT", t) for t in range(NT)]
        if DEBUG:
            dma("sp", DBG_H[b], hT[:].rearrange("p k s -> p (k s)"), HTK, ["dbgh"], "dbgh")
        if STOP == "1a" and b == 0:
            S.emit()
            return nc
        S.barrier(dummy)

        ar = Arena(nc, R_OFF, R_OFF + 106 * KB)
        wqkv = ar.alloc("wqkv", [128, 8, 768], BF16)
        qkT = ar.alloc("qkT", [128, 4, SEQ], BF16)
        vext = ar.alloc("vext", [128, 20, 4, 128], BF16)
        acc = ar.alloc("acc", [128, 4, SEQ], F32)
        den = ar.alloc("den", [64, SEQ // 2], F32)
        NSET = 3
        sqb_l = [ar.alloc("sqb", [128, 512], F32) for _ in range(2)] * 2
        qkn_l = [ar.alloc("qkn", [128, 8, 64], F32) for _ in range(NSET)]
        qkb_l = [ar.alloc("qkb", [128, 8, 64], BF16) for _ in range(NSET)]
        rp_l = [ar.alloc("rp", [128, 4, 8, 8], F32) for _ in range(NSET)]
        s8_l = [ar.alloc("s8", [128, 8], F32) for _ in range(NSET)]
        PT = [ar.alloc("PT", [128, 2, 256], BF16) for _ in range(4)]
        ptc = [0]
        mset("pool", vext[:, :, :, 64:128], 1.0, ["vext1"])

        for g in range(3):
            dil = DIL[g]
            L = SEQ // dil
            nt = L // 128
            wload(wqkv[:, :, 0:256], WINv[:, :, g * 256:(g + 1) * 256], "wqkv", "wq")
            wload(wqkv[:, :, 256:512], WINv[:, :, 768 + g * 256:768 + (g + 1) * 256], "wqkv", "wk")
            wload(wqkv[:, :, 512:768], WINv[:, :, 1536 + g * 256:1536 + (g + 1) * 256], "wqkv", "wv")
            pipe2, pipe3 = Pipe(1), Pipe(1)
            for i in range(16):
                r_, t_ = i // nt, i % nt
                st = 128 * t_ * dil + r_
                sel = slice(st, st + 127 * dil + 1, dil)
                p, kp = pf(0, 4)
                for k in range(8):
                    mm(p[:], hT[:, k, sel], wqkv[:, k, 0:512], k == 0, k == 7, HTK + ["wqkv"], [kp])
                z_ = i % NSET
                sqb, qkn, s8 = sqb_l[i % 2], qkn_l[z_], s8_l[z_]
                ksq, ks8, kqn, kqb = ("sqb", i % 2), ("s8", z_), ("qkn", z_), ("qkb", z_)
                act(sqb[:], p[:], AF.Square, [kp], [ksq])
                red(s8[:], sqb[:].rearrange("p (h d) -> p h d", h=8), ALU.add, [ksq], [ks8])
                rsqrt_mean(s8[:], s8[:], 64, [ks8], [ks8])
                tt("dve", qkn[:], p[:].rearrange("p (h d) -> p h d", h=8),
                   s8[:].unsqueeze(2).to_broadcast([128, 8, 64]), ALU.mult, [kp, ks8], [kqn])

                def stage2(i=i, z_=z_, kqn=kqn, kqb=kqb):
                    qkn, qkb, rp = qkn_l[z_], qkb_l[z_], rp_l[z_]
                    tt("dve", qkn[:, :, 0:16], qkn[:, :, 0:16], gqkB[:, :, 0:16], ALU.mult, [kqn] + GQK, [kqn])
                    cp("act", qkb[:], qkn[:], [kqn], [kqb])
                    ti = g * 16 + i
                    cB = cosT[:, ti, :].unsqueeze(1).to_broadcast([128, 8, 8])
                    sB = sinT[:, ti, :].unsqueeze(1).to_broadcast([128, 8, 8])
                    t1 = qkn[:, :, 0:8]
                    t2 = qkn[:, :, 8:16]
                    tt("dve", rp[:, 0], t1, cB, ALU.mult, [kqn, "cosT"], [("rp0", z_)])
                    tt("dve", rp[:, 1], t2, sB, ALU.mult, [kqn, "sinT"], [("rp1", z_)])
                    tt("dve", rp[:, 2], t2, cB, ALU.mult, [kqn, "cosT"], [("rp2", z_)])
                    tt("dve", rp[:, 3], t1, sB, ALU.mult, [kqn, "sinT"], [("rp3", z_)])
                    tt("dve", qkb[:, :, 0:8], rp[:, 0], rp[:, 1], ALU.subtract,
                       [("rp0", z_), ("rp1", z_), kqb], [kqb])
                    tt("dve", qkb[:, :, 8:16], rp[:, 2], rp[:, 3], ALU.add, [("rp2", z_), ("rp3", z_), kqb], [kqb])

                    def stage3(qkb=qkb, kqb=kqb, i=i):
                        pbt, kb = pb()
                        qkf = qkb[:].rearrange("p h d -> p (h d)")
                        for j in range(4):
                            tr(pbt[:, j, :], qkf[:, j * 128:(j + 1) * 128], ident[:], [kqb, "ident"], [kb])
                        act(qkT[:, 0:2, i * 128:(i + 1) * 128], pbt[:, 0:2, :], AF.Copy, [kb], [("qkT", i)],
                            scale=gcol[:, 0:1])
                        act(qkT[:, 2:4, i * 128:(i + 1) * 128], pbt[:, 2:4, :], AF.Copy, [kb, ("qkT", i)], [("qkT", i)],
                            scale=gcol[:, 1:2])
                    pipe3.push(stage3)
                pipe2.push(stage2)
            pipe2.flush()
            pipe3.flush()
            QK = [("qkT", i) for i in range(16)]
            if STOP == "attn_qk":
                S.emit()
                return nc
            vtiles = []
            if g < 2:
                for r_ in range(dil):
                    for j in range(nt + 1):
                        if j == 0:
                            vtiles.append((r_, j, 0, 64))
                        elif j == nt:
                            vtiles.append((r_, j, L - 64, 64))
                        else:
                            vtiles.append((r_, j, 128 * j - 64, 128))
            else:
                for r_ in range(dil):
                    vtiles.append((r_, 0, 0, 128))
            for vi, (r_, j, l0, n) in enumerate(vtiles):
                st = l0 * dil + r_
                sel = slice(st, st + (n - 1) * dil + 1, dil)
                p, kp = pf(0, 4)
                for k in range(8):
                    mm(p[0:n, 0:256], hT[:, k, sel], wqkv[:, k, 512:768], k == 0, k == 7, HTK + ["wqkv"], [kp])
                cp("act", vext[0:n, vi, :, 0:64], p[0:n, 0:256].rearrange("p (s d) -> p s d", s=4),
                   [kp, "vext1"], [("vext", vi)])
            if STOP == "attn_v":
                S.emit()
                return nc
            po = [(PF[4], ("pf", 4)), (PF[5], ("pf", 5))]

            def evac(pot, kpo, r_, t_):
                st = 128 * t_ * dil + r_
                av = acc[:, :, st:st + 127 * dil + 1:dil]
                pv = pot[:].rearrange("p (s q) -> p s q", s=4)
                if g == 0:
                    cp("dve", av, pv, [kpo], ["acc"])
                else:
                    tt("dve", av, pv, av, ALU.add, [kpo, "acc"], ["acc"])

            apipe = Pipe(1)
            for vi, (r_, j, l0, n) in enumerate(vtiles):
                kc0 = r_ * L + l0
                if g == 2:
                    qts, qc0, nq, msk = [0], r_ * L, 128, maskD[:, 0:128]
                elif j == 0:
                    qts, qc0, nq, msk = [0], r_ * L, 128, mask0[0:64, 0:128]
                elif j == nt:
                    qts, qc0, nq, msk = [nt - 1], r_ * L + L - 128, 128, maskG[0:64, 0:128]
                else:
                    qts, qc0, nq, msk = [j - 1, j], r_ * L + 128 * (j - 1), 256, maskG[:, 0:256]
                pts = []
                for half in range(2):
                    p, kp = pf(0, 4)
                    ptile = PT[ptc[0] % 4]
                    pkey = ("PT", ptc[0] % 4)
                    ptc[0] += 1
                    pv3 = p[:].rearrange("p (s q) -> p s q", s=2)
                    for sl_ in range(2):
                        s_ = sl_ * 2 + half
                        pr = slice((s_ % 2) * 64, (s_ % 2) * 64 + 64)
                        mm(pv3[0:n, sl_, 0:nq], qkT[pr, 2 + s_ // 2, kc0:kc0 + n], qkT[pr, s_ // 2, qc0:qc0 + nq],
                           True, True, QK, [kp])
                    if ATT == 1:
                        continue
                    act(ptile[0:n, :, 0:nq], pv3[0:n, :, 0:nq], AF.Exp, [kp], [pkey], scale=0.125)
                    if ATT != 2:
                        tt("dve", ptile[0:n, :, 0:nq], ptile[0:n, :, 0:nq],
                           msk.unsqueeze(1).to_broadcast([n, 2, nq]), ALU.mult, [pkey, "masks"], [pkey])
                    pts.append((ptile, pkey))
                if ATT in (1, 2, 3):
                    continue
                def stage_pv(qts=qts, pts=pts, r_=r_, j=j, n=n, vi=vi):
                    for qi, t_ in enumerate(qts):
                        if g == 2:
                            pot, kpo = po[r_ % 2]
                            first, last = True, True
                        else:
                            pot, kpo = po[t_ % 2]
                            first, last = (j == t_), (j == t_ + 1)
                        pov = pot[:].rearrange("p (s q) -> p s q", s=4)
                        for s_ in range(4):
                            ptile, pkey = pts[s_ % 2]
                            mm(pov[:, s_, :], vext[0:n, vi, s_, :], ptile[0:n, s_ // 2, qi * 128:(qi + 1) * 128],
                               first and s_ == 0, last, [("vext", vi), pkey], [kpo], skip=True)
                        if last and ATT != 4:
                            evac(pot, kpo, r_, t_)
                apipe.push(stage_pv)
            apipe.flush()
        if STOP == "attn_loop":
            S.emit()
            return nc
        rdl = [den[:, 0:512], den[:, 512:1024]]
        for s_ in range(4):
            for n_ in range(4):
                cs = slice(n_ * 512, (n_ + 1) * 512)
                p, kp = pf(0, 4)
                mm(p[0:64, :], identf[:, 64:128], acc[:, s_, cs], True, True, ["acc", "identf"], [kp])
                rd, rk = rdl[n_ % 2], ("rd", n_ % 2)
                act(rd, p[0:64, :], AF.Ln, [kp], [rk])
                act(rd, rd, AF.Exp, [rk], [rk], scale=-1.0)
                tt("dve", oaT[:, s_, cs], acc[0:64, s_, cs], rd, ALU.mult, ["acc", rk], [("oaT", s_)])
        if DEBUG:
            dma("sp", DBG_OA[b], oaT[:].rearrange("p k s -> p (k s)"), [("oaT", s_) for s_ in range(4)],
                ["dbgoa"], "dbgoa")
        if STOP == "attn" and b == 0:
            S.emit()
            return nc
        S.barrier(dummy)

        ar = Arena(nc, R_OFF, R_OFF + 88 * KB)
        wkv = ar.alloc("wkv", [128, 8, 1024], BF16)
        wqm = ar.alloc("wqm", [128, 8, 512], BF16)
        memT = ar.alloc("memT", [128, 8, 256], BF16)
        kmT = ar.alloc("kmT", [128, 4, 256], BF16)
        vm = ar.alloc("vm", [128, 2, 512], BF16)
        qmT = ar.alloc("qmT", [128, 4, SEQ], BF16)
        MSET = 3
        sqm_l = [ar.alloc("sqm", [128, 512], F32) for _ in range(MSET)]
        qn_l = [ar.alloc("qn", [128, 4, 128], F32) for _ in range(MSET)]
        qnb_l = [ar.alloc("qnb", [128, 4, 128], BF16) for _ in range(MSET)]
        s4_l = [ar.alloc("s4", [128, 4], F32) for _ in range(MSET)]
        hnc = [0]
        PM = [ar.alloc("PM", [128, 512], BF16) for _ in range(4)]
        rden_l = [ar.alloc("rden", [128, 512], F32) for _ in range(2)]
        wload(wkv[:], W_MKV[0].rearrange("(k p) n -> p k n", p=128), "wkv", "wkv")
        wload(wqm[:], WINv[:, :, 4608:5120], "wqm", "wqm")
        npipe = Pipe(1)
        for m in range(2):
            norm_rows(MEM[b, m * 128:(m + 1) * 128, :], gmemB, "gmemB", memT, m * 128, m, m, 4 + m, ("memT", m))
        npipe.flush()
        MT = [("memT", 0), ("memT", 1)]

        def headnorm(p, kp):
            z_ = hnc[0] % MSET
            hnc[0] += 1
            sqm, qn, s4 = sqm_l[z_], qn_l[z_], s4_l[z_]
            act(sqm[:], p[:], AF.Square, [kp], [("sqm", z_)])
            red(s4[:], sqm[:].rearrange("p (h d) -> p h d", h=4), ALU.add, [("sqm", z_)], [("s4", z_)])
            rsqrt_mean(s4[:], s4[:], 128, [("s4", z_)], [("s4", z_)])
            tt("dve", qn[:], p[:].rearrange("p (h d) -> p h d", h=4),
               s4[:].unsqueeze(2).to_broadcast([128, 4, 128]), ALU.mult, [kp, ("s4", z_)], [("qn", z_)])
            return z_

        def headgain(z_, gB, gkey):
            cp("act", qnb_l[z_][:], qn_l[z_][:], [("qn", z_)], [("qnb", z_)])
            return qnb_l[z_], ("qnb", z_)

        for m in range(2):
            p, kp = pf()
            for k in range(8):
                mm(p[:], memT[:, k, m * 128:(m + 1) * 128], wkv[:, k, 0:512], k == 0, k == 7, MT + ["wkv"], [kp])
            qnb, kqnb = headgain(headnorm(p, kp), gkmB, "gkmB")
            pbt, kb = pb()
            for hh in range(4):
                tr(pbt[:, hh, :], qnb[:, hh, :], ident[:], [kqnb, "ident"], [kb])
            act(kmT[:, :, m * 128:(m + 1) * 128], pbt[:, 0:4, :], AF.Copy, [kb], [("kmT", m)], scale=gcol[:, 3:4])
            p, kp = pf()
            for k in range(8):
                mm(p[:], memT[:, k, m * 128:(m + 1) * 128], wkv[:, k, 512:1024], k == 0, k == 7, MT + ["wkv"], [kp])
            cp("act", vm[:, m, :], p[:], [kp], [("vm", m)])
        mpipe, mpipe3 = Pipe(1), Pipe(1)
        for t in range(NT):
            p, kp = pf()
            for k in range(8):
                mm(p[:], hT[:, k, t * 128:(t + 1) * 128], wqm[:, k, :], k == 0, k == 7, HTK + ["wqm"], [kp])
            zq = headnorm(p, kp)

            def stage2(zq=zq, t=t):
                qnb, kqnb = headgain(zq, gqmB, "gqmB")

                def stage3(qnb=qnb, kqnb=kqnb, t=t):
                    pbt, kb = pb()
                    for hh in range(4):
                        tr(pbt[:, hh, :], qnb[:, hh, :], ident[:], [kqnb, "ident"], [kb])
                    act(qmT[:, :, t * 128:(t + 1) * 128], pbt[:, 0:4, :], AF.Copy, [kb], [("qmT", t)],
                        scale=gcol[:, 2:3])
                mpipe3.push(stage3)
            mpipe.push(stage2)
        mpipe.flush()
        mpipe3.flush()
        QM = [("qmT", t) for t in range(NT)]
        KM = [("kmT", 0), ("kmT", 1), ("vm", 0), ("vm", 1)]
        pmc = 0
        mapipe = Pipe(1)
        for hh in range(4):
            for n_ in range(4):
                cs = slice(n_ * 512, (n_ + 1) * 512)
                pms = []
                for m in range(2):
                    p, kp = pf()
                    mm(p[:], kmT[:, hh, m * 128:(m + 1) * 128], qmT[:, hh, cs], True, True, QM + KM, [kp])
                    pm_, pmk = PM[pmc % 4], ("PM", pmc % 4)
                    pmc += 1
                    act(pm_[:], p[:], AF.Exp, [kp], [pmk], scale=float(128 ** -0.5))
                    pms.append((pm_, pmk))

                def stage_b(pms=pms, hh=hh, n_=n_, cs=cs):
                    pnum, knum = pf()
                    pden, kden = pf()
                    for m in range(2):
                        pm_, pmk = pms[m]
                        mm(pnum[:], vm[:, m, hh * 128:(hh + 1) * 128], pm_[:], m == 0, m == 1, KM + [pmk], [knum])
                        mm(pden[:], ones_b[:], pm_[:], m == 0, m == 1, ["ones_b", pmk], [kden])
                    rd_, rdk = rden_l[n_ % 2], ("rden", n_ % 2)
                    act(rd_[:], pden[:], AF.Ln, [kden], [rdk])
                    act(rd_[:], rd_[:], AF.Exp, [rdk], [rdk], scale=-1.0)
                    tt("dve", omT[:, hh, cs], pnum[:], rd_[:], ALU.mult, [knum, rdk], [("omT", hh, n_)])
                mapipe.push(stage_b)
        mapipe.flush()
        if DEBUG:
            dma("sp", DBG_OM[b], omT[:].rearrange("p k s -> p (k s)"),
                [("omT", hh, n_) for hh in range(4) for n_ in range(4)], ["dbgom"], "dbgom")
        if STOP == "mem" and b == 0:
            S.emit()
            return nc
        S.barrier(dummy)

        ar = Arena(nc, R_OFF, R_OFF + 64 * KB)
        wcv = [ar.alloc("wcv", [128, 8, 3, 128], BF16) for _ in range(2)]
        cxs = [ar.alloc("cxs", [128, 512], F32) for _ in range(2)]
        U = ar.alloc("U", [128, SEQ + 2], F32)
        ycv = ar.alloc("ycv", [128, SEQ], F32)
        mset("dve", U[:, 0:1], 0.0, ["U0"])
        mset("dve", U[:, SEQ + 1:SEQ + 2], 0.0, ["U1"])
        for c in range(6):
            w_ = wcv[c % 2]
            wk = ("wcv", c % 2)
            for j, base in enumerate((2304, 3072, 3840)):
                wload(w_[:, :, j, :], WINv[:, :, base + c * 128:base + (c + 1) * 128], wk, ("wcv", c % 2, j))
            for n_ in range(4):
                cs = slice(n_ * 512, (n_ + 1) * 512)
                p, kp = pf()
                for k in range(8):
                    mm(p[:], w_[:, k, 0, :], hT[:, k, cs], k == 0, k == 7, HTK + [wk], [kp])
                cx_, cxk = cxs[n_ % 2], ("cxs", n_ % 2)
                cp("act", cx_[:], p[:], [kp], [cxk])
                p, kp = pf()
                for k in range(8):
                    mm(p[:], w_[:, k, 2, :], hT[:, k, cs], k == 0, k == 7, HTK + [wk], [kp])
                tt("dve", U[:, 1 + n_ * 512:1 + (n_ + 1) * 512], p[:], cx_[:], ALU.mult, [kp, cxk, "ycv"],
                   [("U", n_)])
            UK = [("U", n_) for n_ in range(4)] + ["U0", "U1"]
            ts("dve", ycv[:], U[:, 0:SEQ], wcol[:, c:c + 1], None, ALU.mult, None, UK + ["wcol"], ["ycv"])
            stt("dve", ycv[:], U[:, 1:SEQ + 1], wcol[:, 6 + c:7 + c], ycv[:], ALU.mult, ALU.add,
                UK + ["wcol", "ycv"], ["ycv"])
            stt("dve", ycv[:], U[:, 2:SEQ + 2], wcol[:, 12 + c:13 + c], ycv[:], ALU.mult, ALU.add,
                UK + ["wcol", "ycv"], ["ycv"])
            for n_ in range(4):
                cs = slice(n_ * 512, (n_ + 1) * 512)
                p, kp = pf()
                for k in range(8):
                    mm(p[:], w_[:, k, 1, :], hT[:, k, cs], k == 0, k == 7, HTK + [wk], [kp])
                tt("dve", zT[:, c, cs], p[:], ycv[:, cs], ALU.mult, [kp, "ycv"], [("zT", c, n_)])
        if DEBUG:
            dma("sp", DBG_Z[b], zT[:].rearrange("p k s -> p (k s)"),
                [("zT", c, n_) for c in range(6) for n_ in range(4)], ["dbgz"], "dbgz")
        if STOP == "conv" and b == 0:
            S.emit()
            return nc
        S.barrier(dummy)

        ar = Arena(nc, R_OFF, R_OFF + 32 * KB)
        wgt_ = [ar.alloc("wgate", [128, 8, 3, 128], BF16) for _ in range(2)]
        wpa = [ar.alloc("wpa", [64, 4, 128], BF16) for _ in range(2)]
        wpc = [ar.alloc("wpc", [128, 6, 128], BF16) for _ in range(2)]
        wpm = [ar.alloc("wpm", [128, 4, 128], BF16) for _ in range(2)]
        sg = [ar.alloc("sg", [128, 512], F32) for _ in range(3)]
        mtmp = [ar.alloc("mtmp", [128, 512], F32) for _ in range(2)]
        for c in range(8):
            sl = c % 2
            dsl = slice(c * 128, (c + 1) * 128)
            for j in range(3):
                wload(wgt_[sl][:, :, j, :], WINv[:, :, 5120 + j * 1024 + c * 128:5120 + j * 1024 + (c + 1) * 128],
                      ("wgate", sl), ("wgate", sl, j))
            wload(wpa[sl][:], W_PA[0].rearrange("(s p) n -> p s n", p=64)[:, :, dsl], ("wpa", sl), ("wpa", sl))
            wload(wpc[sl][:], W_PC[0].rearrange("(k p) n -> p k n", p=128)[:, :, dsl], ("wpc", sl), ("wpc", sl))
            wload(wpm[sl][:], W_PM[0].rearrange("(k p) n -> p k n", p=128)[:, :, dsl], ("wpm", sl), ("wpm", sl))
            for n_ in range(4):
                cs = slice(n_ * 512, (n_ + 1) * 512)
                for j in range(3):
                    p, kp = pf()
                    for k in range(8):
                        mm(p[:], wgt_[sl][:, k, j, :], hT[:, k, cs], k == 0, k == 7, HTK + [("wgate", sl)], [kp])
                    act(sg[j][:], p[:], AF.Sigmoid, [kp], [("sg", j)])
                pa, kpa = pf()
                for s_ in range(4):
                    mm(pa[:], wpa[sl][:, s_, :], oaT[:, s_, cs], s_ == 0, s_ == 3,
                       [("wpa", sl)] + [("oaT", q) for q in range(4)], [kpa])
                tt("dve", mtmp[0][:], pa[:], sg[0][:], ALU.mult, [kpa, ("sg", 0)], ["mtmp0"])
                pc_, kpc = pf()
                for k in range(6):
                    mm(pc_[:], wpc[sl][:, k, :], zT[:, k, cs], k == 0, k == 5,
                       [("wpc", sl)] + [("zT", q, n_) for q in range(6)], [kpc])
                tt("dve", mtmp[1][:], pc_[:], sg[1][:], ALU.mult, [kpc, ("sg", 1)], ["mtmp1"])
                tt("dve", mtmp[0][:], mtmp[0][:], mtmp[1][:], ALU.add, ["mtmp0", "mtmp1"], ["mtmp0"])
                pm2, kpm = pf()
                for k in range(4):
                    mm(pm2[:], wpm[sl][:, k, :], omT[:, k, cs], k == 0, k == 3,
                       [("wpm", sl)] + [("omT", q, n_) for q in range(4)], [kpm])
                tt("dve", mtmp[1][:], pm2[:], sg[2][:], ALU.mult, [kpm, ("sg", 2), "mtmp1"], ["mtmp1"])
                tt("dve", mT[:, c, cs], mtmp[0][:], mtmp[1][:], ALU.add, ["mtmp0", "mtmp1"], [("mT", n_)])
        if DEBUG:
            dma("sp", DBG_M[b], mT[:].rearrange("p k s -> p (k s)"), [("mT", n_) for n_ in range(4)],
                ["dbgm"], "dbgm")
        if STOP == "merge" and b == 0:
            S.emit()
            return nc
        S.barrier(dummy)

        ar = Arena(nc, R_OFF + 64 * KB, R_END)
        wout = ar.alloc("wout", [128, 8, D], BF16)
        lgA = ar.alloc("lgA", [128, NT, 36], F32)
        loop_base = ar.off
        x1t = [ar.alloc("x1t", [128, D], F32) for _ in range(2)]
        h2f = [ar.alloc("h2f", [128, D], F32) for _ in range(2)]
        h2b = [ar.alloc("h2b", [128, D], BF16) for _ in range(2)]
        h2T = [ar.alloc("h2T", [128, 8, 128], F32) for _ in range(2)]
        wload(wout[:], W_OUT[0].rearrange("(k p) n -> p k n", p=128), "wout", "wout")
        xpipe, xpipe_c = Pipe(1), Pipe(1)
        for t in range(NT):
            gt = b * NT + t
            sl = t % 2
            row0 = b * SEQ + t * 128
            if t == 0:
                dma("sp", xt[0][:], X[b, 0:128, :], [], [("xt", 0)], ("xt", 0))
            if t + 1 < NT:
                dma("sp", xt[(t + 1) % 2][:], X[b, (t + 1) * 128:(t + 2) * 128, :], [], [("xt", (t + 1) % 2)],
                    ("xt", (t + 1) % 2))
            for h_ in range(2):
                p, kp = pf(0, 4)
                for k in range(8):
                    mm(p[:], mT[:, k, t * 128:(t + 1) * 128], wout[:, k, h_ * 512:(h_ + 1) * 512], k == 0, k == 7,
                       [("mT", t // 4), "wout"], [kp])
                tt("dve", x1t[sl][:, h_ * 512:(h_ + 1) * 512], p[:], xt[sl][:, h_ * 512:(h_ + 1) * 512], ALU.add,
                   [kp, ("xt", sl)], [("x1t", sl, h_)])
            XK = [("x1t", sl, 0), ("x1t", sl, 1)]
            dma("sp", X1S[row0:row0 + 128, :], x1t[sl][:], XK, [("x1s", gt)], ("x1s", sl))
            sc = 8 + sl
            sq = stat[:, sc:sc + 1]
            hk = ("h2f", sl)
            act(h2f[sl][:], x1t[sl][:], AF.Square, XK, [hk, ("stat", sc)], accum=sq)
            rsqrt_mean(sq, sq, D, [("stat", sc)], [("stat", sc)])
            stt("dve", h2f[sl][:], x1t[sl][:], sq, gffnB[:], ALU.mult, ALU.mult, XK + [("stat", sc), "gffnB"], [hk])
            def stage_b(sl=sl, hk=hk, t=t, gt=gt, row0=row0):
                cp("act", h2b[sl][:], h2f[sl][:], [hk], [("h2b", sl)])
                dma("sp", H2S[row0:row0 + 128, :], h2b[sl][:], [("h2b", sl)], [("h2s", gt)], ("h2s", sl))
                for hf in range(2):
                    p, kp = PF[4 + hf], ("pf", 4 + hf)
                    for k4 in range(4):
                        k = hf * 4 + k4
                        tr(p[:, k4 * 128:(k4 + 1) * 128], h2f[sl][:, k * 128:(k + 1) * 128], identf[:],
                           [hk, "identf"], [kp])
                    cp("dve", h2T[sl][:, hf * 4:(hf + 1) * 4, :],
                       p[:].rearrange("p (k q) -> p k q", k=4), [kp], [("h2T", sl, hf)])

                def stage_c(sl=sl, t=t):
                    pl, kpl = PBF[t % 2], ("pb", t % 2)
                    for k in range(8):
                        mm(pl[:, 0:36], h2T[sl][:, k, :], wr[:, k, :], k == 0, k == 7,
                           [("h2T", sl, 0), ("h2T", sl, 1), "wrg", "wre"], [kpl])
                    tt("dve", lgA[:, t, :], pl[:, 0:36], biasB[:], ALU.add, [kpl, "biasg", "biase"], [("lgA", t)])
                xpipe_c.push(stage_c)
            xpipe.push(stage_b)
        xpipe.flush()
        xpipe_c.flush()
        S.barrier(dummy)
        ar.off = loop_base
        T_ = NT
        def ra(name, shape, dt=F32):
            return ar.alloc(name, shape, dt)
        mx = ra("mx", [128, T_])
        ohg = ra("ohg", [128, T_, 4])
        eg = ra("eg", [128, T_, 4])
        sme = ra("sme", [128, T_])
        pgt = ra("pgt", [128, T_])
        tmp4 = ra("tmp4", [128, T_, 4, 8])
        selv = ra("selv", [128, T_, 8])
        sel2 = ra("sel2", [128, T_, 8])
        s1 = ra("s1", [128, T_])
        s2 = ra("s2", [128, T_])
        e2 = ra("e2", [128, T_])
        rr = ra("rr", [128, T_])
        eq1 = ra("eq1", [128, T_, 8])
        eq2 = ra("eq2", [128, T_, 8])
        oh1 = ra("oh1", [128, T_, 32])
        oh2 = ra("oh2", [128, T_, 32])
        Af = ra("Af", [128, T_, 32])
        Ab = ra("Ab", [128, T_, 32], BF16)
        cs = ra("cs", [128, T_ + 1, 32])
        posb = ra("posb", [128, T_, 32])
        prod = ra("prod", [128, T_, 32])
        dstf = ra("dstf", [128, T_, 2])
        g4 = lgA[:, :, 0:4]
        red(mx[:], g4, ALU.max, [], ["mx"])
        tt("dve", ohg[:], g4, mx[:].unsqueeze(2).to_broadcast([128, T_, 4]), ALU.is_equal, ["mx"], ["ohg"])
        tt("dve", eg[:], g4, mx[:].unsqueeze(2).to_broadcast([128, T_, 4]), ALU.subtract, ["mx"], ["eg"])
        act(eg[:], eg[:], AF.Exp, ["eg"], ["eg"])
        red(sme[:], eg[:], ALU.add, ["eg"], ["sme"])
        recip(pgt[:], sme[:], ["sme"], ["pgt"])
        tt("dve", tmp4[:], lgA[:, :, 4:36].rearrange("p t (g e) -> p t g e", g=4),
           ohg[:].unsqueeze(3).to_broadcast([128, T_, 4, 8]), ALU.mult, ["ohg"], ["tmp4"])
        red(selv[:], tmp4[:].rearrange("p t g e -> p t e g"), ALU.add, ["tmp4"], ["selv"])
        red(s1[:], selv[:], ALU.max, ["selv"], ["s1"])
        tt("dve", eq1[:], selv[:], s1[:].unsqueeze(2).to_broadcast([128, T_, 8]), ALU.is_equal, ["selv", "s1"], ["eq1"])
        stt("dve", sel2[:], eq1[:], -1e30, selv[:], ALU.mult, ALU.add, ["eq1", "selv"], ["sel2"])
        red(s2[:], sel2[:], ALU.max, ["sel2"], ["s2"])
        tt("dve", eq2[:], sel2[:], s2[:].unsqueeze(2).to_broadcast([128, T_, 8]), ALU.is_equal, ["sel2", "s2"], ["eq2"])
        tt("dve", e2[:], s2[:], s1[:], ALU.subtract, ["s1", "s2"], ["e2"])
        act(e2[:], e2[:], AF.Exp, ["e2"], ["e2"])
        ts("dve", rr[:], e2[:], 1.0, None, ALU.add, None, ["e2"], ["rr"])
        recip(rr[:], rr[:], ["rr"], ["rr"])
        w0v = wgt[:, b * NT:(b + 1) * NT, 0]
        w1v = wgt[:, b * NT:(b + 1) * NT, 1]
        tt("dve", w0v, pgt[:], rr[:], ALU.mult, ["pgt", "rr"], ["w0v"])
        tt("dve", w1v, w0v, e2[:], ALU.mult, ["w0v", "e2"], ["w1v"])
        for (ohq, eqq, kq) in ((oh1, eq1, "oh1"), (oh2, eq2, "oh2")):
            tt("dve", ohq[:].rearrange("p t (g e) -> p t g e", g=4),
               ohg[:].unsqueeze(3).to_broadcast([128, T_, 4, 8]),
               eqq[:].unsqueeze(2).to_broadcast([128, T_, 4, 8]), ALU.mult, ["ohg", "eq1", "eq2"], [kq])
        tt("dve", Af[:], oh1[:], oh2[:], ALU.add, ["oh1", "oh2"], ["Af"])
        cp("dve", Ab[:], Af[:], ["Af"], ["Ab"])
        pp, kpp = pf()
        for t in range(T_):
            mm(pp[:, t * 32:(t + 1) * 32], utri[:], Ab[:, t, :], True, True, ["Ab"], [kpp])
        pc, kpc2 = pf()
        mm(pc[:], ones_b[:], Ab[:].rearrange("p t e -> p (t e)"), True, True, ["Ab"], [kpc2])
        cp("dve", cs[:, 0, :], srun[:], [], ["cs"])
        for t in range(T_):
            tt("dve", cs[:, t + 1, :], cs[:, t, :], pc[:, t * 32:(t + 1) * 32], ALU.add, ["cs", kpc2], ["cs"])
        cp("dve", srun[:], cs[:, T_, :], ["cs"], ["srun"])
        tt("dve", posb[:], pp[:].rearrange("p (t e) -> p t e", t=T_), cs[:, 0:T_, :], ALU.add, [kpp, "cs"], ["posb"])
        tt("dve", posb[:], posb[:], ecap[:].unsqueeze(1).to_broadcast([128, T_, 32]), ALU.add, ["posb"], ["posb"])
        for q, ohq in enumerate((oh1, oh2)):
            tt("dve", prod[:], ohq[:], posb[:], ALU.mult, ["oh1", "oh2", "posb"], ["prod"])
            red(dstf[:, :, q], prod[:], ALU.add, ["prod"], ["dstf"])
        ts("dve", dstf[:], dstf[:], float(NROWS - 1), None, ALU.min, None, ["dstf"], ["dstf"])
        cp("dve", dest[:, b * NT:(b + 1) * NT, :], dstf[:], ["dstf"], ["dest"])
        for t in range(T_):
            gt = b * NT + t
            for q in range(2):
                scatter(ROWTOK, dest[:, gt, q:q + 1], tok[:, gt:gt + 1], ["dest"], [("rowtok", gt, q)], ("scat", q))
        if STOP == "x1" and b == 0:
            S.emit()
            return nc
        S.barrier(dummy)

    ar = Arena(nc, XT_OFF, R_END)
    NW = 3
    wg_ = [ar.alloc("wg", [128, 8, 512], BF16) for _ in range(NW)]
    wu_ = [ar.alloc("wu", [128, 8, 512], BF16) for _ in range(NW)]
    wd_ = [ar.alloc("wd", [128, 4, D], BF16) for _ in range(NW)]
    idxe = [ar.alloc("idxe", [128, 3], I32) for _ in range(2)]
    xg = [ar.alloc("xg", [128, 3, D], BF16) for _ in range(2)]
    xgT = [ar.alloc("xgT", [128, 8, CAP], BF16) for _ in range(2)]
    sa = [ar.alloc("sa", [128, CAP], F32) for _ in range(2)]
    actT = [ar.alloc("actT", [128, 4, CAP], BF16) for _ in range(2)]
    yt = [ar.alloc("yt", [128, D], F32) for _ in range(3)]
    cb0 = [ar.alloc("cb0", [128, D], F32) for _ in range(2)]
    cy0 = [ar.alloc("cy0", [128, D], F32) for _ in range(2)]
    cy1 = [ar.alloc("cy1", [128, D], F32) for _ in range(2)]
    ytc = [0]

    def load_w(ex):
        ws = ex % NW
        wload(wg_[ws][:], W_G[0, ex].rearrange("(k p) n -> p k n", p=128), ("wg", ws), ("wg", ws))
        wload(wu_[ws][:], W_U[0, ex].rearrange("(k p) n -> p k n", p=128), ("wu", ws), ("wu", ws))
        wload(wd_[ws][:], W_D[0, ex].rearrange("(k p) n -> p k n", p=128), ("wd", ws), ("wd", ws))

    def load_rows(ex):
        sl = ex % 2
        dma("sp", idxe[sl][:], ROWTOK[ex * CAP:(ex + 1) * CAP, :].rearrange("(p j) o -> p (j o)", p=128),
            [], [("idxe", sl)], ("idxe", sl))
        for j in range(3):
            gather(xg[sl][:, j, :], H2S, idxe[sl][:, j:j + 1], [("idxe", sl)], [("xg", sl, j)], ("xg", sl, j), NTOK)

    load_rows(0)
    for ex0 in range(NW):
        load_w(ex0)
    epipe = Pipe(1)
    for ex in range(NEXP):
        sl = ex % 2
        ws = ex % NW
        if ex + 1 < NEXP:
            load_rows(ex + 1)
        for j in range(3):
            pbt, kb = pb()
            for k in range(8):
                tr(pbt[:, k, :], xg[sl][:, j, k * 128:(k + 1) * 128], ident[:], [("xg", sl, j), "ident"], [kb])
            cp("act" if j % 2 == 0 else "dve", xgT[sl][:, :, j * 128:(j + 1) * 128], pbt[:], [kb], [("xgT", sl, j)])
        XG = [("xgT", sl, j) for j in range(3)]
        for f in range(4):
            pa, kpa = pf()
            for k in range(8):
                mm(pa[:, 0:CAP], wg_[ws][:, k, f * 128:(f + 1) * 128], xgT[sl][:, k, :], k == 0, k == 7,
                   XG + [("wg", ws)], [kpa])
            pu, kpu = pf()
            for k in range(8):
                mm(pu[:, 0:CAP], wu_[ws][:, k, f * 128:(f + 1) * 128], xgT[sl][:, k, :], k == 0, k == 7,
                   XG + [("wu", ws)], [kpu])
            act(sa[f % 2][:], pa[:, 0:CAP], AF.Silu, [kpa], [("sa", f % 2)])
            tt("dve", actT[sl][:, f, :], pu[:, 0:CAP], sa[f % 2][:], ALU.mult, [kpu, ("sa", f % 2)],
               [("actT", sl, f)])
        AK = [("actT", sl, f) for f in range(4)]

        def stage_b(sl=sl, ws=ws, ex=ex, AK=AK):
            for j in range(3):
                y_, yk = yt[ytc[0] % 3], ("yt", ytc[0] % 3)
                ytc[0] += 1
                for h_ in range(2):
                    p, kp = pf()
                    for f in range(4):
                        mm(p[:], actT[sl][:, f, j * 128:(j + 1) * 128], wd_[ws][:, f, h_ * 512:(h_ + 1) * 512],
                           f == 0, f == 3, AK + [("wd", ws)], [kp])
                    cp("act" if h_ == 0 else "dve", y_[:, h_ * 512:(h_ + 1) * 512], p[:], [kp], [(yk, h_)])
                ysv = YS[ex * CAP:(ex + 1) * CAP, :].rearrange("(p j) d -> p j d", j=3)[:, j, :]
                dma("sp", ysv, y_[:], [(yk, 0), (yk, 1)], [("ys", ex, j)], yk)
            if ex + NW < NEXP:
                load_w(ex + NW)
        epipe.push(stage_b)
    epipe.flush()
    S.barrier(dummy)
    if STOP == "moe":
        S.emit()
        return nc

    for gt in range(NTOK // 128):
        sl = gt % 2
        dma("sp", cb0[sl][:], X1S[gt * 128:(gt + 1) * 128, :], [], [("cb0", sl)], ("cb0", sl))
        gather(cy0[sl][:], YS, dest[:, gt, 0:1], [], [("cy0", sl)], ("cy0", sl), NROWS)
        gather(cy1[sl][:], YS, dest[:, gt, 1:2], [], [("cy1", sl)], ("cy1", sl), NROWS)
        stt("dve", cb0[sl][:], cy0[sl][:], wgt[:, gt, 0:1], cb0[sl][:], ALU.mult, ALU.add,
            [("cb0", sl), ("cy0", sl)], [("cb0", sl)])
        stt("dve", cb0[sl][:], cy1[sl][:], wgt[:, gt, 1:2], cb0[sl][:], ALU.mult, ALU.add,
            [("cb0", sl), ("cy1", sl)], [("cb0", sl)])
        dma("sp", OUT[gt * 128:(gt + 1) * 128, :], cb0[sl][:], [("cb0", sl)], [("out", gt)], ("outst", sl))
    S.emit()
    return nc


_NC_CACHE = {}


def kernel(**inputs):
    if "nc" not in _NC_CACHE:
        _NC_CACHE["nc"] = build_nc()
    nc = _NC_CACHE["nc"]
    in_maps = []
    for c in range(NCORES):
        m = {}
        for k, v in inputs.items():
            v = np.asarray(v)
            if k in ("x", "mem", "positions"):
                m[k] = np.ascontiguousarray(v[NSEQ * c:NSEQ * (c + 1)])
            else:
                m[k] = np.ascontiguousarray(v)
        in_maps.append(m)
    res = run_bass_kernel_spmd(nc, in_maps, core_ids=list(range(NCORES)))
    kernel.last = res
    out = np.concatenate([np.asarray(r["out"]).reshape(NSEQ, SEQ, D) for r in res.results], axis=0)
    return out.astype(np.float32)
```

```python
import numpy as np
import concourse.bass as bass
import concourse.mybir as mybir
from concourse.bass_utils import run_bass_kernel_spmd

F32 = mybir.dt.float32
BF16 = mybir.dt.bfloat16
I32 = mybir.dt.int32
ALU = mybir.AluOpType
AF = mybir.ActivationFunctionType
AX = mybir.AxisListType

NCORES = 8
SEQ = 2048
D = 1024
NSEQ = 2
NTOK = NSEQ * SEQ
NT = SEQ // 128
DIL = (1, 4, 16)
EPS = 1e-6
NEXP = 32
CAP = 384
NROWS = NEXP * CAP
DEBUG = False
STOP = None
ATT = 0


class Sched:
    ENG = ("pe", "act", "dve", "pool", "sp")

    def __init__(self, nc):
        self.nc = nc
        self.ops = {e: [] for e in self.ENG}
        self.res = {}
        self.dma_keys = {}
        self.all_ops = []

    def _r(self, key):
        r = self.res.get(key)
        if r is None:
            r = self.res[key] = [None, []]
        return r

    def add(self, eng, fn, reads=(), writes=(), dma_key=None, extra_deps=()):
        op = dict(eng=eng, fn=fn, dma_key=dma_key, signal=False, cnt=0)
        deps = list(extra_deps)
        for k in reads:
            r = self._r(k)
            if r[0] is not None:
                deps.append(r[0])
        for k in writes:
            r = self._r(k)
            if r[0] is not None:
                deps.append(r[0])
            deps.extend(r[1])
        for k in reads:
            self._r(k)[1].append(op)
        for k in writes:
            r = self._r(k)
            r[0] = op
            r[1] = []
        op["deps"] = deps
        if dma_key is not None:
            lst = self.dma_keys.setdefault(dma_key, [])
            lst.append(op)
            op["cnt"] = 16 * len(lst)
        self.ops[eng].append(op)
        self.all_ops.append(op)
        return op

    def barrier(self, dummy):
        last = []
        for e in self.ENG:
            for op in reversed(self.ops[e]):
                if op["dma_key"] is None and op["fn"] is not None:
                    last.append(op)
                    break
        for k, lst in self.dma_keys.items():
            last.append(lst[-1])
        self.res = {}
        for e in self.ENG:
            self.add(e, None, extra_deps=last)

    def emit(self):
        nc = self.nc
        for op in self.all_ops:
            for d in op["deps"]:
                if d["dma_key"] is None and not (d["eng"] == op["eng"] == "pe"):
                    d["signal"] = True
        esem = {e: nc.alloc_semaphore("sem_" + e) for e in self.ENG}
        dsem = {k: nc.alloc_semaphore("dsem_%d" % i) for i, k in enumerate(self.dma_keys)}
        for e in self.ENG:
            c = 0
            for op in self.ops[e]:
                if op["dma_key"] is None and op["signal"]:
                    assert op["fn"] is not None
                    c += 1
                    op["cnt"] = c
        with nc.Block() as block:
            def run(ename, eng):
                waited = {}
                for op in self.ops[ename]:
                    need = {}
                    for d in op["deps"]:
                        if d["dma_key"] is not None:
                            s = dsem[d["dma_key"]]
                        else:
                            if d["eng"] == ename == "pe":
                                continue
                            s = esem[d["eng"]]
                        if d["cnt"] > need.get(s, 0):
                            need[s] = d["cnt"]
                    for s, v in need.items():
                        if waited.get(s, 0) < v:
                            eng.wait_ge(s, v)
                            waited[s] = v
                    if op["fn"] is None:
                        continue
                    ins = op["fn"](eng)
                    if op["dma_key"] is not None:
                        ins.then_inc(dsem[op["dma_key"]], 16)
                    elif op["signal"]:
                        ins.then_inc(esem[ename], 1)
                if ename == "sp":
                    for k, lst in self.dma_keys.items():
                        eng.wait_ge(dsem[k], 16 * len(lst))

            @block.tensor
            def _(eng):
                run("pe", eng)

            @block.scalar
            def _(eng):
                run("act", eng)

            @block.vector
            def _(eng):
                run("dve", eng)

            @block.gpsimd
            def _(eng):
                run("pool", eng)

            @block.sync
            def _(eng):
                run("sp", eng)


def _bytes(shape, dt):
    n = 1
    for s in shape[1:]:
        n *= s
    sz = {F32: 4, BF16: 2, I32: 4}[dt]
    return (n * sz + 31) // 32 * 32


class Arena:
    def __init__(self, nc, base, limit):
        self.nc, self.base, self.limit, self.off = nc, base, limit, base
        self.n = 0

    def alloc(self, name, shape, dt):
        b = _bytes(shape, dt)
        assert self.off + b <= self.limit, (name, self.off, b, self.limit)
        t = self.nc.alloc_sbuf_tensor_at("%s_%d" % (name, Arena.uid()), list(shape), dt, offset=self.off)
        self.off += b
        return t

    _uid = [0]

    @staticmethod
    def uid():
        Arena._uid[0] += 1
        return Arena._uid[0]

    def reset(self):
        self.off = self.base


class Pipe:
    def __init__(self, depth=1):
        self.q, self.depth = [], depth

    def push(self, fn):
        self.q.append(fn)
        while len(self.q) > self.depth:
            self.q.pop(0)()

    def flush(self):
        while self.q:
            self.q.pop(0)()


def build_nc():
    nc = bass.Bass("TRN2", target_bir_lowering=False)
    S = Sched(nc)

    def din(name, shape, dt=F32):
        return nc.dram_tensor(name, list(shape), dt, kind="ExternalInput").ap()

    X = din("x", [NSEQ, SEQ, D])
    MEM = din("mem", [NSEQ, 256, D])
    POS = din("positions", [NSEQ, SEQ], I32)
    G_MIX = din("g_mix", [1, D])
    G_MEM = din("g_mem", [1, D])
    W_IN = din("w_in", [1, D, 8192])
    G_QA = din("g_qn_attn", [1, 64])
    G_KA = din("g_kn_attn", [1, 64])
    W_CONV = din("w_conv", [1, 3, 768])
    W_MKV = din("w_mem_kv", [1, D, 1024])
    G_QM = din("g_qn_mem", [1, 128])
    G_KM = din("g_kn_mem", [1, 128])
    W_PA = din("w_proj_attn", [1, 256, D])
    W_PC = din("w_proj_conv", [1, 768, D])
    W_PM = din("w_proj_mem", [1, 512, D])
    W_OUT = din("w_out", [1, D, D])
    G_FFN = din("g_ffn", [1, D])
    W_RG = din("w_router_group", [1, D, 4])
    B_RG = din("b_router_group", [1, 4])
    W_RE = din("w_router_expert", [1, D, 32])
    B_RE = din("b_router_expert", [1, 32])
    nexp_decl = 1 if (STOP is not None and STOP != "moe") else NEXP
    W_G = din("w_gate", [1, nexp_decl, D, 512])
    W_U = din("w_up", [1, nexp_decl, D, 512])
    W_D = din("w_down", [1, nexp_decl, 512, D])
    OUT = nc.dram_tensor("out", [NTOK, D], F32, kind="ExternalOutput").ap()
    skind = "ExternalOutput" if DEBUG else "Internal"
    X1S = nc.dram_tensor("x1s", [NTOK, D], F32, kind=skind).ap()
    H2S = nc.dram_tensor("h2s", [NTOK, D], BF16, kind=skind).ap()
    ROWTOK = nc.dram_tensor("rowtok", [NROWS + 1024, 1], I32, kind=skind).ap()
    YS = nc.dram_tensor("ys", [NROWS, D], F32, kind=skind).ap()
    if DEBUG:
        DBG_OA = nc.dram_tensor("dbg_oa", [NSEQ, 64, 4 * SEQ], BF16, kind="ExternalOutput").ap()
        DBG_Z = nc.dram_tensor("dbg_z", [NSEQ, 128, 6 * SEQ], BF16, kind="ExternalOutput").ap()
        DBG_OM = nc.dram_tensor("dbg_om", [NSEQ, 128, 4 * SEQ], BF16, kind="ExternalOutput").ap()
        DBG_M = nc.dram_tensor("dbg_m", [NSEQ, 128, 8 * SEQ], BF16, kind="ExternalOutput").ap()
        DBG_H = nc.dram_tensor("dbg_h", [NSEQ, 128, 8 * SEQ], BF16, kind="ExternalOutput").ap()

    WINv = W_IN[0].rearrange("(k p) n -> p k n", p=128)

    def mm(out, lhsT, rhs, start, stop, r, w, skip=False):
        if skip:
            S.add("pe", lambda e: e.matmul(out, lhsT=lhsT, rhs=rhs, start=start, stop=stop,
                                           skip_group_check=True), reads=r, writes=w)
        else:
            S.add("pe", lambda e: e.matmul(out, lhsT=lhsT, rhs=rhs, start=start, stop=stop), reads=r, writes=w)

    def tr(out, in_, ident, r, w):
        S.add("pe", lambda e: e.transpose(out=out, in_=in_, identity=ident), reads=r, writes=w)

    def act(out, in_, func, r, w, scale=None, bias=None, accum=None):
        def f(e):
            kw = {}
            if scale is not None:
                kw["scale"] = scale
            if bias is not None:
                kw["bias"] = bias
            if accum is not None:
                kw["accum_out"] = accum
            return e.activation(out=out, in_=in_, func=func, **kw)
        S.add("act", f, reads=r, writes=w)

    def tt(eng, out, in0, in1, op, r, w):
        S.add(eng, lambda e: e.tensor_tensor(out=out, in0=in0, in1=in1, op=op), reads=r, writes=w)

    def ts(eng, out, in0, s1, s2, op0, op1, r, w):
        if op1 is None:
            S.add(eng, lambda e: e.tensor_scalar(out=out, in0=in0, scalar1=s1, scalar2=None, op0=op0),
                  reads=r, writes=w)
        else:
            S.add(eng, lambda e: e.tensor_scalar(out=out, in0=in0, scalar1=s1, scalar2=s2, op0=op0, op1=op1),
                  reads=r, writes=w)

    def stt(eng, out, in0, scalar, in1, op0, op1, r, w):
        S.add(eng, lambda e: e.scalar_tensor_tensor(out=out, in0=in0, scalar=scalar, in1=in1, op0=op0, op1=op1),
              reads=r, writes=w)

    def cp(eng, out, in_, r, w):
        if eng == "act":
            S.add("act", lambda e: e.activation(out=out, in_=in_, func=AF.Copy), reads=r, writes=w)
        else:
            S.add(eng, lambda e: e.tensor_copy(out=out, in_=in_), reads=r, writes=w)

    def red(out, in_, op, r, w):
        S.add("dve", lambda e: e.tensor_reduce(out=out, in_=in_, axis=AX.X, op=op), reads=r, writes=w)

    def recip(out, in_, r, w):
        S.add("dve", lambda e: e.reciprocal(out=out, in_=in_), reads=r, writes=w)

    def mset(eng, ap, val, w):
        S.add(eng, lambda e: e.memset(ap, val), writes=w)

    def dma(eng, out, in_, r, w, key, slow=False):
        if slow:
            S.add(eng, lambda e: e.dma_start(out=out, in_=in_, allow_slow_non_contiguous=True),
                  reads=r, writes=w, dma_key=key)
        else:
            S.add(eng, lambda e: e.dma_start(out=out, in_=in_), reads=r, writes=w, dma_key=key)

    def gather(out, src, idx, r, w, key, nrows):
        S.add("pool", lambda e: e.indirect_dma_start(
            out=out, out_offset=None, in_=src,
            in_offset=bass.IndirectOffsetOnAxis(ap=idx, axis=0)), reads=r, writes=w, dma_key=key)

    def scatter(dst, idx, src, r, w, key):
        S.add("pool", lambda e: e.indirect_dma_start(
            out=dst, out_offset=bass.IndirectOffsetOnAxis(ap=idx, axis=0), in_=src, in_offset=None),
            reads=r, writes=w, dma_key=key)

    def rsqrt_mean(out, ssq, n, r, w):
        act(out, ssq, AF.Sqrt, list(r) + ["eps_c"], w, scale=1.0 / n, bias=eps_c[:, 0:1])
        recip(out, out, list(w), w)

    PF = [nc.alloc_psum_tensor("pf%d" % i, [128, 512], F32) for i in range(6)]
    PB = [nc.alloc_psum_tensor("pb%d" % i, [128, 8, 128], BF16) for i in range(2)]
    PBF = [PB[i][:].rearrange("p k q -> p (k q)").bitcast(F32) for i in range(2)]
    rot = {"f": 0, "b": 0}

    def pf(lo=0, hi=6):
        i = lo + rot["f"] % (hi - lo)
        rot["f"] += 1
        return PF[i], ("pf", i)

    def pb():
        i = rot["b"] % 2
        rot["b"] += 1
        return PB[i], ("pb", i)

    KB = 1024
    BASE = 17 * KB
    CA = Arena(nc, BASE, BASE + 32 * KB)
    ident = CA.alloc("ident", [128, 128], BF16)
    identf = CA.alloc("identf", [128, 128], F32)
    tmpf = CA.alloc("tmpf", [128, 256], F32)
    maskG = CA.alloc("maskG", [128, 256], BF16)
    mask0 = CA.alloc("mask0", [128, 128], BF16)
    maskD = CA.alloc("maskD", [128, 128], BF16)
    utri = CA.alloc("utri", [128, 128], BF16)
    ones_b = CA.alloc("ones_b", [128, 128], BF16)
    gmixB = CA.alloc("gmixB", [128, D], F32)
    gffnB = CA.alloc("gffnB", [128, D], F32)
    gmemB = CA.alloc("gmemB", [128, D], F32)
    gqkB = CA.alloc("gqkB", [128, 8, 64], F32)
    gqmB = CA.alloc("gqmB", [128, 128], F32)
    gkmB = CA.alloc("gkmB", [128, 128], F32)
    wc18 = CA.alloc("wc18", [18, 128], F32)
    wcol = CA.alloc("wcol", [128, 18], F32)
    invf = CA.alloc("invf", [128, 8], F32)
    posi = CA.alloc("posi", [128, 3, 16], I32)
    posf = CA.alloc("posf", [128, 3, 16], F32)
    ang = CA.alloc("ang", [128, 48, 8], F32)
    cosT = CA.alloc("cosT", [128, 48, 8], F32)
    sinT = CA.alloc("sinT", [128, 48, 8], F32)
    angk = CA.alloc("angk", [128, 48, 8], F32)
    angi = CA.alloc("angi", [128, 48, 8], I32)
    wr = CA.alloc("wr", [128, 8, 36], F32)
    biasB = CA.alloc("biasB", [128, 36], F32)
    ecap = CA.alloc("ecap", [128, 32], F32)
    tok = CA.alloc("tok", [128, 32], I32)
    dest = CA.alloc("dest", [128, 32, 2], I32)
    wgt = CA.alloc("wgt", [128, 32, 2], F32)
    srun = CA.alloc("srun", [128, 32], F32)
    srunb = CA.alloc("srunb", [128, 32], BF16)
    stat = CA.alloc("stat", [128, 64], F32)
    dummy = CA.alloc("dummy", [128, 8], F32)
    zero_i = CA.alloc("zero_i", [128, NROWS // 128], I32)
    pi_c = CA.alloc("pi_c", [128, 1], F32)
    eps_c = CA.alloc("eps_c", [128, 1], F32)
    gcol = CA.alloc("gcol", [128, 4], F32)
    XT_OFF = BASE + 32 * KB
    HT_OFF = BASE + 52 * KB
    R_OFF = BASE + 84 * KB
    R_END = BASE + 206 * KB
    xa = Arena(nc, XT_OFF, HT_OFF)
    xt = [xa.alloc("xt", [128, D], F32) for _ in range(4)]
    hb = [xa.alloc("hb", [128, D], BF16) for _ in range(2)]
    hT = nc.alloc_sbuf_tensor_at("hT", [128, 8, SEQ], BF16, offset=HT_OFF)
    oaT = nc.alloc_sbuf_tensor_at("oaT", [64, 4, SEQ], BF16, offset=R_OFF + 106 * KB)
    omT = nc.alloc_sbuf_tensor_at("omT", [128, 4, SEQ], BF16, offset=R_OFF + 88 * KB)
    zT = nc.alloc_sbuf_tensor_at("zT", [128, 6, SEQ], BF16, offset=R_OFF + 64 * KB)
    mT = nc.alloc_sbuf_tensor_at("mT", [128, 8, SEQ], BF16, offset=R_OFF + 32 * KB)

    def bcast_load(dst, src_row, n, key):
        dma("sp", dst, src_row.partition_broadcast(128), [], [key], key)

    bcast_load(gmixB[:], G_MIX[0:1, :], D, "gmixB")
    bcast_load(gffnB[:], G_FFN[0:1, :], D, "gffnB")
    bcast_load(gmemB[:], G_MEM[0:1, :], D, "gmemB")
    for hh in range(4):
        bcast_load(gqkB[:, hh, :], G_QA[0:1, :], 64, ("gqk", hh))
        bcast_load(gqkB[:, 4 + hh, :], G_KA[0:1, :], 64, ("gqk", 4 + hh))
    bcast_load(gqmB[:], G_QM[0:1, :], 128, "gqmB")
    bcast_load(gkmB[:], G_KM[0:1, :], 128, "gkmB")
    bcast_load(biasB[:, 0:4], B_RG[0:1, :], 4, "biasg")
    bcast_load(biasB[:, 4:36], B_RE[0:1, :], 32, "biase")
    GQK = [("gqk", i) for i in range(8)]
    for hh in range(2):
        dma("sp", gcol[hh * 64:(hh + 1) * 64, 0:1], G_QA.rearrange("o n -> n o"), [], [("gcq", hh)], ("gcq", hh), slow=True)
        dma("sp", gcol[hh * 64:(hh + 1) * 64, 1:2], G_KA.rearrange("o n -> n o"), [], [("gck", hh)], ("gck", hh), slow=True)
        mset("dve", gcol[hh * 64:hh * 64 + 16, 0:2], 1.0, [("gcq", hh), ("gck", hh)])
    dma("sp", gcol[:, 2:3], G_QM.rearrange("o n -> n o"), [], ["gcqm"], "gcqm", slow=True)
    dma("sp", gcol[:, 3:4], G_KM.rearrange("o n -> n o"), [], ["gckm"], "gckm", slow=True)
    dma("sp", wc18[:], W_CONV[0].rearrange("t (c p) -> (t c) p", p=128), [], ["wc18"], "wc18")
    dma("sp", wr[:, :, 0:4], W_RG[0].rearrange("(k p) n -> p k n", p=128), [], ["wrg"], "wrg")
    dma("sp", wr[:, :, 4:36], W_RE[0].rearrange("(k p) n -> p k n", p=128), [], ["wre"], "wre")

    S.add("pool", lambda e: e.memset(identf[:], 0.0), writes=["identf"])
    S.add("pool", lambda e: e.affine_select(out=identf[:], in_=identf[:], pattern=[[-1, 128]],
                                            compare_op=ALU.not_equal, fill=1.0, base=0, channel_multiplier=1),
          reads=["identf"], writes=["identf"])
    cp("dve", ident[:], identf[:], ["identf"], ["ident"])

    def build_mask(dst, rows, cols, conds):
        S.add("pool", lambda e: e.memset(tmpf[:, 0:cols], 1.0), writes=["tmpf"])
        for (base, cm, step) in conds:
            S.add("pool", lambda e, base=base, cm=cm, step=step: e.affine_select(
                out=tmpf[:, 0:cols], in_=tmpf[:, 0:cols], pattern=[[step, cols]],
                compare_op=ALU.is_ge, fill=0.0, base=base, channel_multiplier=cm),
                reads=["tmpf"], writes=["tmpf"])
        cp("dve", dst, tmpf[:, 0:cols], ["tmpf"], ["masks"])

    build_mask(maskG[:], 128, 256, [(0, -1, 1), (128, 1, -1)])
    build_mask(mask0[:], 128, 128, [(64, 1, -1)])
    build_mask(maskD[:], 128, 128, [(64, -1, 1), (64, 1, -1)])
    build_mask(utri[:], 128, 128, [(-1, -1, 1)])
    mset("dve", ones_b[:], 1.0, ["ones_b"])
    mset("dve", pi_c[:], float(np.pi), ["pi_c"])
    mset("dve", eps_c[:], EPS, ["eps_c"])
    mset("dve", srun[:], 0.0, ["srun"])
    mset("dve", srunb[:], 0.0, ["srunb"])
    mset("dve", zero_i[:], 0, ["zero_i"])
    for j in range(8):
        v = float(np.float32(500000.0) ** np.float32(-j / 8.0))
        mset("dve", invf[:, j:j + 1], v, ["invf"])
    S.add("pool", lambda e: e.iota(tok[:], [[128, 32]], base=0, channel_multiplier=1), writes=["tok"])
    S.add("pool", lambda e: e.iota(ecap[:], [[CAP, 32]], base=0, channel_multiplier=0,
                                   allow_small_or_imprecise_dtypes=True), writes=["ecap"])
    pft, kft = pf()
    tr(pft[:, 0:18], wc18[:], identf[0:18, 0:18], ["wc18", "identf"], [kft])
    cp("dve", wcol[:], pft[:, 0:18], [kft], ["wcol"])
    dma("sp", ROWTOK[0:NROWS, :].rearrange("(p j) o -> p (j o)", p=128), zero_i[:], ["zero_i"], ["rowtok0"], "rowtok0")

    S.barrier(dummy)
    if STOP == "const":
        S.emit()
        return nc

    def wload(dst, src, wkey, dkey):
        dma("pool", dst, src, [], [wkey], dkey)

    for b in range(NSEQ):
        pv0 = POS[b].rearrange("(t p) -> p t", p=128)
        pv1 = POS[b].rearrange("(t p r) -> p r t", t=4, p=128, r=4)
        pv2 = POS[b].rearrange("(p r) -> p r", r=16)
        dma("sp", posi[:, 0, :], pv0, [], [("posi", 0)], ("posi", 0), slow=True)
        dma("sp", posi[:, 1, :].rearrange("p (r t) -> p r t", r=4), pv1, [], [("posi", 1)], ("posi", 1), slow=True)
        dma("sp", posi[:, 2, :], pv2, [], [("posi", 2)], ("posi", 2), slow=True)
        PK = [("posi", i) for i in range(3)]
        cp("dve", posf[:], posi[:], PK, ["posf"])
        pfl = posf[:].rearrange("p g t -> p (g t)")
        tt("dve", ang[:], pfl.unsqueeze(2).to_broadcast([128, 48, 8]),
           invf[:].unsqueeze(1).to_broadcast([128, 48, 8]), ALU.mult, ["posf", "invf"], ["ang"])
        def sin_of(dst, shift):
            ts("dve", angk[:], ang[:], shift, 1.0 / (2 * np.pi), ALU.add, ALU.mult, ["ang"], ["angk"])
            cp("dve", angi[:], angk[:], ["angk"], ["angi"])
            cp("dve", angk[:], angi[:], ["angi"], ["angk"])
            ts("dve", dst, ang[:], shift, None, ALU.add, None, ["ang"], ["sdst"])
            stt("dve", dst, angk[:], -2 * np.pi, dst, ALU.mult, ALU.add, ["angk", "sdst"], ["sdst"])
            ts("dve", angk[:], dst, float(np.pi), None, ALU.is_gt, None, ["sdst"], ["angk"])
            stt("dve", dst, angk[:], -2 * np.pi, dst, ALU.mult, ALU.add, ["angk", "sdst"], ["sdst"])
            ts("dve", dst, dst, -float(np.pi), float(np.pi), ALU.max, ALU.min, ["sdst"], ["sdst"])
            act(dst, dst, AF.Sin, ["sdst"], ["sdst"])

        sin_of(sinT[:], 0.0)
        sin_of(cosT[:], float(np.pi / 2))
        def norm_rows(src_ap, gB, gkey, dstT, dcol0, xs, hs, statcol, dkey):
            dma("sp", xt[xs][:], src_ap, [], [("xt", xs)], ("xt", xs))
            sq = stat[:, statcol:statcol + 1]
            act(hb[hs][:], xt[xs][:], AF.Square, [("xt", xs)], [("hb", hs), ("stat", statcol)], accum=sq)
            rsqrt_mean(sq, sq, D, [("stat", statcol)], [("stat", statcol)])
            stt("dve", hb[hs][:], xt[xs][:], sq, gB[:], ALU.mult, ALU.mult,
                [("xt", xs), ("stat", statcol), gkey], [("hb", hs)])
            pbt, kb = pb()
            for k in range(8):
                tr(pbt[:, k, :], hb[hs][:, k * 128:(k + 1) * 128], ident[:], [("hb", hs), "ident"], [kb])
            npipe.push(lambda pbt=pbt, kb=kb: cp("dve", dstT[:, :, dcol0:dcol0 + 128], pbt[:], [kb], [dkey]))

        npipe = Pipe(1)
        for t in range(NT):
            norm_rows(X[b, t * 128:(t + 1) * 128, :], gmixB, "gmixB", hT, t * 128, t % 4, t % 2, t % 4, ("hT", t))
        npipe.flush()
        HTK = [("hT", t) for t in range(NT)]
        if DEBUG:
            dma("sp", DBG_H[b], hT[:].rearrange("p k s -> p (k s)"), HTK, ["dbgh"], "dbgh")
        if STOP == "1a" and b == 0:
            S.emit()
            return nc
        S.barrier(dummy)

        ar = Arena(nc, R_OFF, R_OFF + 106 * KB)
        wqkv = ar.alloc("wqkv", [128, 8, 768], BF16)
        qkT = ar.alloc("qkT", [128, 4, SEQ], BF16)
        vext = ar.alloc("vext", [128, 20, 4, 128], BF16)
        acc = ar.alloc("acc", [128, 4, SEQ], F32)
        den = ar.alloc("den", [64, SEQ // 2], F32)
        sqb_l = [ar.alloc("sqb", [128, 512], F32) for _ in range(2)]
        qkn_l = [ar.alloc("qkn", [128, 8, 64], F32) for _ in range(2)]
        qkb_l = [ar.alloc("qkb", [128, 8, 64], BF16) for _ in range(4)]
        rp_l = [ar.alloc("rp", [128, 4, 8, 8], F32) for _ in range(4)]
        s8_l = [ar.alloc("s8", [128, 8], F32) for _ in range(2)]
        PT = [ar.alloc("PT", [128, 2, 256], BF16) for _ in range(4)]
        ptc = [0]
        mset("pool", vext[:, :, :, 64:128], 1.0, ["vext1"])

        for g in range(3):
            dil = DIL[g]
            L = SEQ // dil
            nt = L // 128
            wload(wqkv[:, :, 0:256], WINv[:, :, g * 256:(g + 1) * 256], "wqkv", "wq")
            wload(wqkv[:, :, 256:512], WINv[:, :, 768 + g * 256:768 + (g + 1) * 256], "wqkv", "wk")
            wload(wqkv[:, :, 512:768], WINv[:, :, 1536 + g * 256:1536 + (g + 1) * 256], "wqkv", "wv")
            pipe3 = Pipe(1)
            for ia in range(0, 16, 2):
                ctx = []
                for pos in range(2):
                    i = ia + pos
                    r_, t_ = i // nt, i % nt
                    st = 128 * t_ * dil + r_
                    p, kp = pf(0, 4)
                    ctx.append(dict(i=i, pos=pos, z4=i % 4, sel=slice(st, st + 127 * dil + 1, dil), p=p, kp=kp))
                for c in ctx:
                    for k in range(8):
                        mm(c["p"][:], hT[:, k, c["sel"]], wqkv[:, k, 0:512], k == 0, k == 7, HTK + ["wqkv"], [c["kp"]])
                for c in ctx:
                    act(sqb_l[c["pos"]][:], c["p"][:], AF.Square, [c["kp"]], [("sqb", c["pos"])])
                for c in ctx:
                    red(s8_l[c["pos"]][:], sqb_l[c["pos"]][:].rearrange("p (h d) -> p h d", h=8), ALU.add,
                        [("sqb", c["pos"])], [("s8", c["pos"])])
                for c in ctx:
                    act(s8_l[c["pos"]][:], s8_l[c["pos"]][:], AF.Sqrt, [("s8", c["pos"]), "eps_c"], [("s8", c["pos"])],
                        scale=1.0 / 64, bias=eps_c[:, 0:1])
                for c in ctx:
                    recip(s8_l[c["pos"]][:], s8_l[c["pos"]][:], [("s8", c["pos"])], [("s8", c["pos"])])
                for c in ctx:
                    tt("dve", qkn_l[c["pos"]][:], c["p"][:].rearrange("p (h d) -> p h d", h=8),
                       s8_l[c["pos"]][:].unsqueeze(2).to_broadcast([128, 8, 64]), ALU.mult,
                       [c["kp"], ("s8", c["pos"])], [("qkn", c["pos"])])
                for c in ctx:
                    qkn = qkn_l[c["pos"]]
                    tt("dve", qkn[:, :, 0:16], qkn[:, :, 0:16], gqkB[:, :, 0:16], ALU.mult,
                       [("qkn", c["pos"])] + GQK, [("qkn", c["pos"])])
                for c in ctx:
                    cp("act", qkb_l[c["z4"]][:], qkn_l[c["pos"]][:], [("qkn", c["pos"])], [("qkb", c["z4"])])
                for c in ctx:
                    qkn, rp = qkn_l[c["pos"]], rp_l[c["z4"]]
                    ti = g * 16 + c["i"]
                    cB = cosT[:, ti, :].unsqueeze(1).to_broadcast([128, 8, 8])
                    sB = sinT[:, ti, :].unsqueeze(1).to_broadcast([128, 8, 8])
                    t1 = qkn[:, :, 0:8]
                    t2 = qkn[:, :, 8:16]
                    kqn = ("qkn", c["pos"])
                    tt("dve", rp[:, 0], t1, cB, ALU.mult, [kqn, "cosT"], [("rp0", c["z4"])])
                    tt("dve", rp[:, 1], t2, sB, ALU.mult, [kqn, "sinT"], [("rp1", c["z4"])])
                    tt("dve", rp[:, 2], t2, cB, ALU.mult, [kqn, "cosT"], [("rp2", c["z4"])])
                    tt("dve", rp[:, 3], t1, sB, ALU.mult, [kqn, "sinT"], [("rp3", c["z4"])])
                for c in ctx:
                    qkb, rp, z4 = qkb_l[c["z4"]], rp_l[c["z4"]], c["z4"]
                    tt("dve", qkb[:, :, 0:8], rp[:, 0], rp[:, 1], ALU.subtract,
                       [("rp0", z4), ("rp1", z4), ("qkb", z4)], [("qkb", z4)])
                    tt("dve", qkb[:, :, 8:16], rp[:, 2], rp[:, 3], ALU.add,
                       [("rp2", z4), ("rp3", z4), ("qkb", z4)], [("qkb", z4)])

                def stage3(ctx=ctx):
                    outs = []
                    for c in ctx:
                        pbt, kb = pb()
                        qkf = qkb_l[c["z4"]][:].rearrange("p h d -> p (h d)")
                        for j in range(4):
                            tr(pbt[:, j, :], qkf[:, j * 128:(j + 1) * 128], ident[:], [("qkb", c["z4"]), "ident"], [kb])
                        outs.append((pbt, kb, c["i"]))
                    for (pbt, kb, i) in outs:
                        act(qkT[:, 0:2, i * 128:(i + 1) * 128], pbt[:, 0:2, :], AF.Copy, [kb], [("qkT", i)],
                            scale=gcol[:, 0:1])
                    for (pbt, kb, i) in outs:
                        act(qkT[:, 2:4, i * 128:(i + 1) * 128], pbt[:, 2:4, :], AF.Copy, [kb, ("qkT", i)], [("qkT", i)],
                            scale=gcol[:, 1:2])
                pipe3.push(stage3)
            pipe3.flush()
            QK = [("qkT", i) for i in range(16)]
            if STOP == "attn_qk":
                S.emit()
                return nc
            vtiles = []
            if g < 2:
                for r_ in range(dil):
                    for j in range(nt + 1):
                        if j == 0:
                            vtiles.append((r_, j, 0, 64))
                        elif j == nt:
                            vtiles.append((r_, j, L - 64, 64))
                        else:
                            vtiles.append((r_, j, 128 * j - 64, 128))
            else:
                for r_ in range(dil):
                    vtiles.append((r_, 0, 0, 128))
            for vi, (r_, j, l0, n) in enumerate(vtiles):
                st = l0 * dil + r_
                sel = slice(st, st + (n - 1) * dil + 1, dil)
                p, kp = pf(0, 4)
                for k in range(8):
                    mm(p[0:n, 0:256], hT[:, k, sel], wqkv[:, k, 512:768], k == 0, k == 7, HTK + ["wqkv"], [kp])
                cp("act", vext[0:n, vi, :, 0:64], p[0:n, 0:256].rearrange("p (s d) -> p s d", s=4),
                   [kp, "vext1"], [("vext", vi)])
            if STOP == "attn_v":
                S.emit()
                return nc
            po = [(PF[4], ("pf", 4)), (PF[5], ("pf", 5))]

            def evac(pot, kpo, r_, t_):
                st = 128 * t_ * dil + r_
                av = acc[:, :, st:st + 127 * dil + 1:dil]
                pv = pot[:].rearrange("p (s q) -> p s q", s=4)
                if g == 0:
                    cp("dve", av, pv, [kpo], ["acc"])
                else:
                    tt("dve", av, pv, av, ALU.add, [kpo, "acc"], ["acc"])

            apipe = Pipe(1)
            for vi, (r_, j, l0, n) in enumerate(vtiles):
                kc0 = r_ * L + l0
                if g == 2:
                    qts, qc0, nq, msk = [0], r_ * L, 128, maskD[:, 0:128]
                elif j == 0:
                    qts, qc0, nq, msk = [0], r_ * L, 128, mask0[0:64, 0:128]
                elif j == nt:
                    qts, qc0, nq, msk = [nt - 1], r_ * L + L - 128, 128, maskG[0:64, 0:128]
                else:
                    qts, qc0, nq, msk = [j - 1, j], r_ * L + 128 * (j - 1), 256, maskG[:, 0:256]
                pts = []
                for half in range(2):
                    p, kp = pf(0, 4)
                    ptile = PT[ptc[0] % 4]
                    pkey = ("PT", ptc[0] % 4)
                    ptc[0] += 1
                    pv3 = p[:].rearrange("p (s q) -> p s q", s=2)
                    for sl_ in range(2):
                        s_ = sl_ * 2 + half
                        pr = slice((s_ % 2) * 64, (s_ % 2) * 64 + 64)
                        mm(pv3[0:n, sl_, 0:nq], qkT[pr, 2 + s_ // 2, kc0:kc0 + n], qkT[pr, s_ // 2, qc0:qc0 + nq],
                           True, True, QK, [kp])
                    if ATT == 1:
                        continue
                    act(ptile[0:n, :, 0:nq], pv3[0:n, :, 0:nq], AF.Exp, [kp], [pkey], scale=0.125)
                    if ATT != 2:
                        tt("dve", ptile[0:n, :, 0:nq], ptile[0:n, :, 0:nq],
                           msk.unsqueeze(1).to_broadcast([n, 2, nq]), ALU.mult, [pkey, "masks"], [pkey])
                    pts.append((ptile, pkey))
                if ATT in (1, 2, 3):
                    continue
                def stage_pv(qts=qts, pts=pts, r_=r_, j=j, n=n, vi=vi):
                    for qi, t_ in enumerate(qts):
                        if g == 2:
                            pot, kpo = po[r_ % 2]
                            first, last = True, True
                        else:
                            pot, kpo = po[t_ % 2]
                            first, last = (j == t_), (j == t_ + 1)
                        pov = pot[:].rearrange("p (s q) -> p s q", s=4)
                        for s_ in range(4):
                            ptile, pkey = pts[s_ % 2]
                            mm(pov[:, s_, :], vext[0:n, vi, s_, :], ptile[0:n, s_ // 2, qi * 128:(qi + 1) * 128],
                               first and s_ == 0, last, [("vext", vi), pkey], [kpo], skip=True)
                        if last and ATT != 4:
                            evac(pot, kpo, r_, t_)
                apipe.push(stage_pv)
            apipe.flush()
        if STOP == "attn_loop":
            S.emit()
            return nc
        rdl = [den[:, 0:512], den[:, 512:1024]]
        for s_ in range(4):
            for n_ in range(4):
                cs = slice(n_ * 512, (n_ + 1) * 512)
                p, kp = pf(0, 4)
                mm(p[0:64, :], identf[:, 64:128], acc[:, s_, cs], True, True, ["acc", "identf"], [kp])
                rd, rk = rdl[n_ % 2], ("rd", n_ % 2)
                act(rd, p[0:64, :], AF.Ln, [kp], [rk])
                act(rd, rd, AF.Exp, [rk], [rk], scale=-1.0)
                tt("dve", oaT[:, s_, cs], acc[0:64, s_, cs], rd, ALU.mult, ["acc", rk], [("oaT", s_)])
        if DEBUG:
            dma("sp", DBG_OA[b], oaT[:].rearrange("p k s -> p (k s)"), [("oaT", s_) for s_ in range(4)],
                ["dbgoa"], "dbgoa")
        if STOP == "attn" and b == 0:
            S.emit()
            return nc
        S.barrier(dummy)

        ar = Arena(nc, R_OFF, R_OFF + 88 * KB)
        wkv = ar.alloc("wkv", [128, 8, 1024], BF16)
        wqm = ar.alloc("wqm", [128, 8, 512], BF16)
        memT = ar.alloc("memT", [128, 8, 256], BF16)
        kmT = ar.alloc("kmT", [128, 4, 256], BF16)
        vm = ar.alloc("vm", [128, 2, 512], BF16)
        qmT = ar.alloc("qmT", [128, 4, SEQ], BF16)
        MSET = 3
        sqm_l = [ar.alloc("sqm", [128, 512], F32) for _ in range(MSET)]
        qn_l = [ar.alloc("qn", [128, 4, 128], F32) for _ in range(MSET)]
        qnb_l = [ar.alloc("qnb", [128, 4, 128], BF16) for _ in range(MSET)]
        s4_l = [ar.alloc("s4", [128, 4], F32) for _ in range(MSET)]
        hnc = [0]
        PM = [ar.alloc("PM", [128, 512], BF16) for _ in range(4)]
        rden_l = [ar.alloc("rden", [128, 512], F32) for _ in range(2)]
        wload(wkv[:], W_MKV[0].rearrange("(k p) n -> p k n", p=128), "wkv", "wkv")
        wload(wqm[:], WINv[:, :, 4608:5120], "wqm", "wqm")
        npipe = Pipe(1)
        for m in range(2):
            norm_rows(MEM[b, m * 128:(m + 1) * 128, :], gmemB, "gmemB", memT, m * 128, m, m, 4 + m, ("memT", m))
        npipe.flush()
        MT = [("memT", 0), ("memT", 1)]

        def headnorm(p, kp):
            z_ = hnc[0] % MSET
            hnc[0] += 1
            sqm, qn, s4 = sqm_l[z_], qn_l[z_], s4_l[z_]
            act(sqm[:], p[:], AF.Square, [kp], [("sqm", z_)])
            red(s4[:], sqm[:].rearrange("p (h d) -> p h d", h=4), ALU.add, [("sqm", z_)], [("s4", z_)])
            rsqrt_mean(s4[:], s4[:], 128, [("s4", z_)], [("s4", z_)])
            tt("dve", qn[:], p[:].rearrange("p (h d) -> p h d", h=4),
               s4[:].unsqueeze(2).to_broadcast([128, 4, 128]), ALU.mult, [kp, ("s4", z_)], [("qn", z_)])
            return z_

        def headgain(z_, gB, gkey):
            cp("act", qnb_l[z_][:], qn_l[z_][:], [("qn", z_)], [("qnb", z_)])
            return qnb_l[z_], ("qnb", z_)

        for m in range(2):
            p, kp = pf()
            for k in range(8):
                mm(p[:], memT[:, k, m * 128:(m + 1) * 128], wkv[:, k, 0:512], k == 0, k == 7, MT + ["wkv"], [kp])
            qnb, kqnb = headgain(headnorm(p, kp), gkmB, "gkmB")
            pbt, kb = pb()
            for hh in range(4):
                tr(pbt[:, hh, :], qnb[:, hh, :], ident[:], [kqnb, "ident"], [kb])
            act(kmT[:, :, m * 128:(m + 1) * 128], pbt[:, 0:4, :], AF.Copy, [kb], [("kmT", m)], scale=gcol[:, 3:4])
            p, kp = pf()
            for k in range(8):
                mm(p[:], memT[:, k, m * 128:(m + 1) * 128], wkv[:, k, 512:1024], k == 0, k == 7, MT + ["wkv"], [kp])
            cp("act", vm[:, m, :], p[:], [kp], [("vm", m)])
        mpipe, mpipe3 = Pipe(1), Pipe(1)
        for t in range(NT):
            p, kp = pf()
            for k in range(8):
                mm(p[:], hT[:, k, t * 128:(t + 1) * 128], wqm[:, k, :], k == 0, k == 7, HTK + ["wqm"], [kp])
            zq = headnorm(p, kp)

            def stage2(zq=zq, t=t):
                qnb, kqnb = headgain(zq, gqmB, "gqmB")

                def stage3(qnb=qnb, kqnb=kqnb, t=t):
                    pbt, kb = pb()
                    for hh in range(4):
                        tr(pbt[:, hh, :], qnb[:, hh, :], ident[:], [kqnb, "ident"], [kb])
                    act(qmT[:, :, t * 128:(t + 1) * 128], pbt[:, 0:4, :], AF.Copy, [kb], [("qmT", t)],
                        scale=gcol[:, 2:3])
                mpipe3.push(stage3)
            mpipe.push(stage2)
        mpipe.flush()
        mpipe3.flush()
        QM = [("qmT", t) for t in range(NT)]
        KM = [("kmT", 0), ("kmT", 1), ("vm", 0), ("vm", 1)]
        pmc = 0
        mapipe = Pipe(1)
        for hh in range(4):
            for n_ in range(4):
                cs = slice(n_ * 512, (n_ + 1) * 512)
                pms = []
                for m in range(2):
                    p, kp = pf()
                    mm(p[:], kmT[:, hh, m * 128:(m + 1) * 128], qmT[:, hh, cs], True, True, QM + KM, [kp])
                    pm_, pmk = PM[pmc % 4], ("PM", pmc % 4)
                    pmc += 1
                    act(pm_[:], p[:], AF.Exp, [kp], [pmk], scale=float(128 ** -0.5))
                    pms.append((pm_, pmk))

                def stage_b(pms=pms, hh=hh, n_=n_, cs=cs):
                    pnum, knum = pf()
                    pden, kden = pf()
                    for m in range(2):
                        pm_, pmk = pms[m]
                        mm(pnum[:], vm[:, m, hh * 128:(hh + 1) * 128], pm_[:], m == 0, m == 1, KM + [pmk], [knum])
                        mm(pden[:], ones_b[:], pm_[:], m == 0, m == 1, ["ones_b", pmk], [kden])
                    rd_, rdk = rden_l[n_ % 2], ("rden", n_ % 2)
                    act(rd_[:], pden[:], AF.Ln, [kden], [rdk])
                    act(rd_[:], rd_[:], AF.Exp, [rdk], [rdk], scale=-1.0)
                    tt("dve", omT[:, hh, cs], pnum[:], rd_[:], ALU.mult, [knum, rdk], [("omT", hh, n_)])
                mapipe.push(stage_b)
        mapipe.flush()
        if DEBUG:
            dma("sp", DBG_OM[b], omT[:].rearrange("p k s -> p (k s)"),
                [("omT", hh, n_) for hh in range(4) for n_ in range(4)], ["dbgom"], "dbgom")
        if STOP == "mem" and b == 0:
            S.emit()
            return nc
        S.barrier(dummy)

        ar = Arena(nc, R_OFF, R_OFF + 64 * KB)
        wcv = [ar.alloc("wcv", [128, 8, 3, 128], BF16) for _ in range(2)]
        cxs = [ar.alloc("cxs", [128, 512], F32) for _ in range(2)]
        U = ar.alloc("U", [128, SEQ + 2], F32)
        ycv = ar.alloc("ycv", [128, SEQ], F32)
        mset("dve", U[:, 0:1], 0.0, ["U0"])
        mset("dve", U[:, SEQ + 1:SEQ + 2], 0.0, ["U1"])
        for c in range(6):
            w_ = wcv[c % 2]
            wk = ("wcv", c % 2)
            for j, base in enumerate((2304, 3072, 3840)):
                wload(w_[:, :, j, :], WINv[:, :, base + c * 128:base + (c + 1) * 128], wk, ("wcv", c % 2, j))
            for n_ in range(4):
                cs = slice(n_ * 512, (n_ + 1) * 512)
                p, kp = pf()
                for k in range(8):
                    mm(p[:], w_[:, k, 0, :], hT[:, k, cs], k == 0, k == 7, HTK + [wk], [kp])
                cx_, cxk = cxs[n_ % 2], ("cxs", n_ % 2)
                cp("act", cx_[:], p[:], [kp], [cxk])
                p, kp = pf()
                for k in range(8):
                    mm(p[:], w_[:, k, 2, :], hT[:, k, cs], k == 0, k == 7, HTK + [wk], [kp])
                tt("dve", U[:, 1 + n_ * 512:1 + (n_ + 1) * 512], p[:], cx_[:], ALU.mult, [kp, cxk, "ycv"],
                   [("U", n_)])
            UK = [("U", n_) for n_ in range(4)] + ["U0", "U1"]
            ts("dve", ycv[:], U[:, 0:SEQ], wcol[:, c:c + 1], None, ALU.mult, None, UK + ["wcol"], ["ycv"])
            stt("dve", ycv[:], U[:, 1:SEQ + 1], wcol[:, 6 + c:7 + c], ycv[:], ALU.mult, ALU.add,
                UK + ["wcol", "ycv"], ["ycv"])
            stt("dve", ycv[:], U[:, 2:SEQ + 2], wcol[:, 12 + c:13 + c], ycv[:], ALU.mult, ALU.add,
                UK + ["wcol", "ycv"], ["ycv"])
            for n_ in range(4):
                cs = slice(n_ * 512, (n_ + 1) * 512)
                p, kp = pf()
                for k in range(8):
                    mm(p[:], w_[:, k, 1, :], hT[:, k, cs], k == 0, k == 7, HTK + [wk], [kp])
                tt("dve", zT[:, c, cs], p[:], ycv[:, cs], ALU.mult, [kp, "ycv"], [("zT", c, n_)])
        if DEBUG:
            dma("sp", DBG_Z[b], zT[:].rearrange("p k s -> p (k s)"),
                [("zT", c, n_) for c in range(6) for n_ in range(4)], ["dbgz"], "dbgz")
        if STOP == "conv" and b == 0:
            S.emit()
            return nc
        S.barrier(dummy)

        ar = Arena(nc, R_OFF, R_OFF + 32 * KB)
        wgt_ = [ar.alloc("wgate", [128, 8, 3, 128], BF16) for _ in range(2)]
        wpa = [ar.alloc("wpa", [64, 4, 128], BF16) for _ in range(2)]
        wpc = [ar.alloc("wpc", [128, 6, 128], BF16) for _ in range(2)]
        wpm = [ar.alloc("wpm", [128, 4, 128], BF16) for _ in range(2)]
        sg = [ar.alloc("sg", [128, 512], F32) for _ in range(3)]
        mtmp = [ar.alloc("mtmp", [128, 512], F32) for _ in range(2)]
        for c in range(8):
            sl = c % 2
            dsl = slice(c * 128, (c + 1) * 128)
            for j in range(3):
                wload(wgt_[sl][:, :, j, :], WINv[:, :, 5120 + j * 1024 + c * 128:5120 + j * 1024 + (c + 1) * 128],
                      ("wgate", sl), ("wgate", sl, j))
            wload(wpa[sl][:], W_PA[0].rearrange("(s p) n -> p s n", p=64)[:, :, dsl], ("wpa", sl), ("wpa", sl))
            wload(wpc[sl][:], W_PC[0].rearrange("(k p) n -> p k n", p=128)[:, :, dsl], ("wpc", sl), ("wpc", sl))
            wload(wpm[sl][:], W_PM[0].rearrange("(k p) n -> p k n", p=128)[:, :, dsl], ("wpm", sl), ("wpm", sl))
            for n_ in range(4):
                cs = slice(n_ * 512, (n_ + 1) * 512)
                for j in range(3):
                    p, kp = pf()
                    for k in range(8):
                        mm(p[:], wgt_[sl][:, k, j, :], hT[:, k, cs], k == 0, k == 7, HTK + [("wgate", sl)], [kp])
                    act(sg[j][:], p[:], AF.Sigmoid, [kp], [("sg", j)])
                pa, kpa = pf()
                for s_ in range(4):
                    mm(pa[:], wpa[sl][:, s_, :], oaT[:, s_, cs], s_ == 0, s_ == 3,
                       [("wpa", sl)] + [("oaT", q) for q in range(4)], [kpa])
                tt("dve", mtmp[0][:], pa[:], sg[0][:], ALU.mult, [kpa, ("sg", 0)], ["mtmp0"])
                pc_, kpc = pf()
                for k in range(6):
                    mm(pc_[:], wpc[sl][:, k, :], zT[:, k, cs], k == 0, k == 5,
                       [("wpc", sl)] + [("zT", q, n_) for q in range(6)], [kpc])
                tt("dve", mtmp[1][:], pc_[:], sg[1][:], ALU.mult, [kpc, ("sg", 1)], ["mtmp1"])
                tt("dve", mtmp[0][:], mtmp[0][:], mtmp[1][:], ALU.add, ["mtmp0", "mtmp1"], ["mtmp0"])
                pm2, kpm = pf()
                for k in range(4):
                    mm(pm2[:], wpm[sl][:, k, :], omT[:, k, cs], k == 0, k == 3,
                       [("wpm", sl)] + [("omT", q, n_) for q in range(4)], [kpm])
                tt("dve", mtmp[1][:], pm2[:], sg[2][:], ALU.mult, [kpm, ("sg", 2), "mtmp1"], ["mtmp1"])
                tt("dve", mT[:, c, cs], mtmp[0][:], mtmp[1][:], ALU.add, ["mtmp0", "mtmp1"], [("mT", n_)])
        if DEBUG:
            dma("sp", DBG_M[b], mT[:].rearrange("p k s -> p (k s)"), [("mT", n_) for n_ in range(4)],
                ["dbgm"], "dbgm")
        if STOP == "merge" and b == 0:
            S.emit()
            return nc
        S.barrier(dummy)

        ar = Arena(nc, R_OFF + 64 * KB, R_END)
        wout = ar.alloc("wout", [128, 8, D], BF16)
        lgA = ar.alloc("lgA", [128, NT, 36], F32)
        loop_base = ar.off
        x1t = [ar.alloc("x1t", [128, D], F32) for _ in range(2)]
        h2f = [ar.alloc("h2f", [128, D], F32) for _ in range(2)]
        h2b = [ar.alloc("h2b", [128, D], BF16) for _ in range(2)]
        h2T = [ar.alloc("h2T", [128, 8, 128], F32) for _ in range(2)]
        wload(wout[:], W_OUT[0].rearrange("(k p) n -> p k n", p=128), "wout", "wout")
        xpipe, xpipe_c = Pipe(1), Pipe(1)
        for t in range(NT):
            gt = b * NT + t
            sl = t % 2
            row0 = b * SEQ + t * 128
            if t == 0:
                dma("sp", xt[0][:], X[b, 0:128, :], [], [("xt", 0)], ("xt", 0))
            if t + 1 < NT:
                dma("sp", xt[(t + 1) % 2][:], X[b, (t + 1) * 128:(t + 2) * 128, :], [], [("xt", (t + 1) % 2)],
                    ("xt", (t + 1) % 2))
            for h_ in range(2):
                p, kp = pf(0, 4)
                for k in range(8):
                    mm(p[:], mT[:, k, t * 128:(t + 1) * 128], wout[:, k, h_ * 512:(h_ + 1) * 512], k == 0, k == 7,
                       [("mT", t // 4), "wout"], [kp])
                tt("dve", x1t[sl][:, h_ * 512:(h_ + 1) * 512], p[:], xt[sl][:, h_ * 512:(h_ + 1) * 512], ALU.add,
                   [kp, ("xt", sl)], [("x1t", sl, h_)])
            XK = [("x1t", sl, 0), ("x1t", sl, 1)]
            dma("sp", X1S[row0:row0 + 128, :], x1t[sl][:], XK, [("x1s", gt)], ("x1s", sl))
            sc = 8 + sl
            sq = stat[:, sc:sc + 1]
            hk = ("h2f", sl)
            act(h2f[sl][:], x1t[sl][:], AF.Square, XK, [hk, ("stat", sc)], accum=sq)
            rsqrt_mean(sq, sq, D, [("stat", sc)], [("stat", sc)])
            stt("dve", h2f[sl][:], x1t[sl][:], sq, gffnB[:], ALU.mult, ALU.mult, XK + [("stat", sc), "gffnB"], [hk])
            def stage_b(sl=sl, hk=hk, t=t, gt=gt, row0=row0):
                cp("act", h2b[sl][:], h2f[sl][:], [hk], [("h2b", sl)])
                dma("sp", H2S[row0:row0 + 128, :], h2b[sl][:], [("h2b", sl)], [("h2s", gt)], ("h2s", sl))
                for hf in range(2):
                    p, kp = PF[4 + hf], ("pf", 4 + hf)
                    for k4 in range(4):
                        k = hf * 4 + k4
                        tr(p[:, k4 * 128:(k4 + 1) * 128], h2f[sl][:, k * 128:(k + 1) * 128], identf[:],
                           [hk, "identf"], [kp])
                    cp("dve", h2T[sl][:, hf * 4:(hf + 1) * 4, :],
                       p[:].rearrange("p (k q) -> p k q", k=4), [kp], [("h2T", sl, hf)])

                def stage_c(sl=sl, t=t):
                    pl, kpl = PBF[t % 2], ("pb", t % 2)
                    for k in range(8):
                        mm(pl[:, 0:36], h2T[sl][:, k, :], wr[:, k, :], k == 0, k == 7,
                           [("h2T", sl, 0), ("h2T", sl, 1), "wrg", "wre"], [kpl])
                    tt("dve", lgA[:, t, :], pl[:, 0:36], biasB[:], ALU.add, [kpl, "biasg", "biase"], [("lgA", t)])
                xpipe_c.push(stage_c)
            xpipe.push(stage_b)
        xpipe.flush()
        xpipe_c.flush()
        S.barrier(dummy)
        ar.off = loop_base
        T_ = NT
        def ra(name, shape, dt=F32):
            return ar.alloc(name, shape, dt)
        mx = ra("mx", [128, T_])
        ohg = ra("ohg", [128, T_, 4])
        eg = ra("eg", [128, T_, 4])
        sme = ra("sme", [128, T_])
        pgt = ra("pgt", [128, T_])
        tmp4 = ra("tmp4", [128, T_, 4, 8])
        selv = ra("selv", [128, T_, 8])
        sel2 = ra("sel2", [128, T_, 8])
        s1 = ra("s1", [128, T_])
        s2 = ra("s2", [128, T_])
        e2 = ra("e2", [128, T_])
        rr = ra("rr", [128, T_])
        eq1 = ra("eq1", [128, T_, 8])
        eq2 = ra("eq2", [128, T_, 8])
        oh1 = ra("oh1", [128, T_, 32])
        oh2 = ra("oh2", [128, T_, 32])
        Af = ra("Af", [128, T_, 32])
        Ab = ra("Ab", [128, T_, 32], BF16)
        cs = ra("cs", [128, T_ + 1, 32])
        posb = ra("posb", [128, T_, 32])
        prod = ra("prod", [128, T_, 32])
        dstf = ra("dstf", [128, T_, 2])
        g4 = lgA[:, :, 0:4]
        red(mx[:], g4, ALU.max, [], ["mx"])
        tt("dve", ohg[:], g4, mx[:].unsqueeze(2).to_broadcast([128, T_, 4]), ALU.is_equal, ["mx"], ["ohg"])
        tt("dve", eg[:], g4, mx[:].unsqueeze(2).to_broadcast([128, T_, 4]), ALU.subtract, ["mx"], ["eg"])
        act(eg[:], eg[:], AF.Exp, ["eg"], ["eg"])
        red(sme[:], eg[:], ALU.add, ["eg"], ["sme"])
        recip(pgt[:], sme[:], ["sme"], ["pgt"])
        tt("dve", tmp4[:], lgA[:, :, 4:36].rearrange("p t (g e) -> p t g e", g=4),
           ohg[:].unsqueeze(3).to_broadcast([128, T_, 4, 8]), ALU.mult, ["ohg"], ["tmp4"])
        red(selv[:], tmp4[:].rearrange("p t g e -> p t e g"), ALU.add, ["tmp4"], ["selv"])
        red(s1[:], selv[:], ALU.max, ["selv"], ["s1"])
        tt("dve", eq1[:], selv[:], s1[:].unsqueeze(2).to_broadcast([128, T_, 8]), ALU.is_equal, ["selv", "s1"], ["eq1"])
        stt("dve", sel2[:], eq1[:], -1e30, selv[:], ALU.mult, ALU.add, ["eq1", "selv"], ["sel2"])
        red(s2[:], sel2[:], ALU.max, ["sel2"], ["s2"])
        tt("dve", eq2[:], sel2[:], s2[:].unsqueeze(2).to_broadcast([128, T_, 8]), ALU.is_equal, ["sel2", "s2"], ["eq2"])
        tt("dve", e2[:], s2[:], s1[:], ALU.subtract, ["s1", "s2"], ["e2"])
        act(e2[:], e2[:], AF.Exp, ["e2"], ["e2"])
        ts("dve", rr[:], e2[:], 1.0, None, ALU.add, None, ["e2"], ["rr"])
        recip(rr[:], rr[:], ["rr"], ["rr"])
        w0v = wgt[:, b * NT:(b + 1) * NT, 0]
        w1v = wgt[:, b * NT:(b + 1) * NT, 1]
        tt("dve", w0v, pgt[:], rr[:], ALU.mult, ["pgt", "rr"], ["w0v"])
        tt("dve", w1v, w0v, e2[:], ALU.mult, ["w0v", "e2"], ["w1v"])
        for (ohq, eqq, kq) in ((oh1, eq1, "oh1"), (oh2, eq2, "oh2")):
            tt("dve", ohq[:].rearrange("p t (g e) -> p t g e", g=4),
               ohg[:].unsqueeze(3).to_broadcast([128, T_, 4, 8]),
               eqq[:].unsqueeze(2).to_broadcast([128, T_, 4, 8]), ALU.mult, ["ohg", "eq1", "eq2"], [kq])
        tt("dve", Af[:], oh1[:], oh2[:], ALU.add, ["oh1", "oh2"], ["Af"])
        cp("dve", Ab[:], Af[:], ["Af"], ["Ab"])
        pp, kpp = pf()
        for t in range(T_):
            mm(pp[:, t * 32:(t + 1) * 32], utri[:], Ab[:, t, :], True, True, ["Ab"], [kpp])
        pc, kpc2 = pf()
        mm(pc[:], ones_b[:], Ab[:].rearrange("p t e -> p (t e)"), True, True, ["Ab"], [kpc2])
        cp("dve", cs[:, 0, :], srun[:], [], ["cs"])
        for t in range(T_):
            tt("dve", cs[:, t + 1, :], cs[:, t, :], pc[:, t * 32:(t + 1) * 32], ALU.add, ["cs", kpc2], ["cs"])
        cp("dve", srun[:], cs[:, T_, :], ["cs"], ["srun"])
        tt("dve", posb[:], pp[:].rearrange("p (t e) -> p t e", t=T_), cs[:, 0:T_, :], ALU.add, [kpp, "cs"], ["posb"])
        tt("dve", posb[:], posb[:], ecap[:].unsqueeze(1).to_broadcast([128, T_, 32]), ALU.add, ["posb"], ["posb"])
        for q, ohq in enumerate((oh1, oh2)):
            tt("dve", prod[:], ohq[:], posb[:], ALU.mult, ["oh1", "oh2", "posb"], ["prod"])
            red(dstf[:, :, q], prod[:], ALU.add, ["prod"], ["dstf"])
        ts("dve", dstf[:], dstf[:], float(NROWS - 1), None, ALU.min, None, ["dstf"], ["dstf"])
        cp("dve", dest[:, b * NT:(b + 1) * NT, :], dstf[:], ["dstf"], ["dest"])
        for t in range(T_):
            gt = b * NT + t
            for q in range(2):
                scatter(ROWTOK, dest[:, gt, q:q + 1], tok[:, gt:gt + 1], ["dest"], [("rowtok", gt, q)], ("scat", q))
        if STOP == "x1" and b == 0:
            S.emit()
            return nc
        S.barrier(dummy)

    ar = Arena(nc, XT_OFF, R_END)
    NW = 3
    wg_ = [ar.alloc("wg", [128, 8, 512], BF16) for _ in range(NW)]
    wu_ = [ar.alloc("wu", [128, 8, 512], BF16) for _ in range(NW)]
    wd_ = [ar.alloc("wd", [128, 4, D], BF16) for _ in range(NW)]
    idxe = [ar.alloc("idxe", [128, 3], I32) for _ in range(2)]
    xg = [ar.alloc("xg", [128, 3, D], BF16) for _ in range(2)]
    xgT = [ar.alloc("xgT", [128, 8, CAP], BF16) for _ in range(2)]
    sa = [ar.alloc("sa", [128, CAP], F32) for _ in range(2)]
    actT = [ar.alloc("actT", [128, 4, CAP], BF16) for _ in range(2)]
    yt = [ar.alloc("yt", [128, D], F32) for _ in range(3)]
    cb0 = [ar.alloc("cb0", [128, D], F32) for _ in range(2)]
    cy0 = [ar.alloc("cy0", [128, D], F32) for _ in range(2)]
    cy1 = [ar.alloc("cy1", [128, D], F32) for _ in range(2)]
    ytc = [0]

    def load_w(ex):
        ws = ex % NW
        wload(wg_[ws][:], W_G[0, ex].rearrange("(k p) n -> p k n", p=128), ("wg", ws), ("wg", ws))
        wload(wu_[ws][:], W_U[0, ex].rearrange("(k p) n -> p k n", p=128), ("wu", ws), ("wu", ws))
        wload(wd_[ws][:], W_D[0, ex].rearrange("(k p) n -> p k n", p=128), ("wd", ws), ("wd", ws))

    def load_rows(ex):
        sl = ex % 2
        dma("sp", idxe[sl][:], ROWTOK[ex * CAP:(ex + 1) * CAP, :].rearrange("(p j) o -> p (j o)", p=128),
            [], [("idxe", sl)], ("idxe", sl))
        for j in range(3):
            gather(xg[sl][:, j, :], H2S, idxe[sl][:, j:j + 1], [("idxe", sl)], [("xg", sl, j)], ("xg", sl, j), NTOK)

    load_rows(0)
    for ex0 in range(NW):
        load_w(ex0)
    epipe = Pipe(1)
    for ex in range(NEXP):
        sl = ex % 2
        ws = ex % NW
        if ex + 1 < NEXP:
            load_rows(ex + 1)
        for j in range(3):
            pbt, kb = pb()
            for k in range(8):
                tr(pbt[:, k, :], xg[sl][:, j, k * 128:(k + 1) * 128], ident[:], [("xg", sl, j), "ident"], [kb])
            cp("act" if j % 2 == 0 else "dve", xgT[sl][:, :, j * 128:(j + 1) * 128], pbt[:], [kb], [("xgT", sl, j)])
        XG = [("xgT", sl, j) for j in range(3)]
        for f in range(4):
            pa, kpa = pf()
            for k in range(8):
                mm(pa[:, 0:CAP], wg_[ws][:, k, f * 128:(f + 1) * 128], xgT[sl][:, k, :], k == 0, k == 7,
                   XG + [("wg", ws)], [kpa])
            pu, kpu = pf()
            for k in range(8):
                mm(pu[:, 0:CAP], wu_[ws][:, k, f * 128:(f + 1) * 128], xgT[sl][:, k, :], k == 0, k == 7,
                   XG + [("wu", ws)], [kpu])
            act(sa[f % 2][:], pa[:, 0:CAP], AF.Silu, [kpa], [("sa", f % 2)])
            tt("dve", actT[sl][:, f, :], pu[:, 0:CAP], sa[f % 2][:], ALU.mult, [kpu, ("sa", f % 2)],
               [("actT", sl, f)])
        AK = [("actT", sl, f) for f in range(4)]

        def stage_b(sl=sl, ws=ws, ex=ex, AK=AK):
            for j in range(3):
                y_, yk = yt[ytc[0] % 3], ("yt", ytc[0] % 3)
                ytc[0] += 1
                for h_ in range(2):
                    p, kp = pf()
                    for f in range(4):
                        mm(p[:], actT[sl][:, f, j * 128:(j + 1) * 128], wd_[ws][:, f, h_ * 512:(h_ + 1) * 512],
                           f == 0, f == 3, AK + [("wd", ws)], [kp])
                    cp("act" if h_ == 0 else "dve", y_[:, h_ * 512:(h_ + 1) * 512], p[:], [kp], [(yk, h_)])
                ysv = YS[ex * CAP:(ex + 1) * CAP, :].rearrange("(p j) d -> p j d", j=3)[:, j, :]
                dma("sp", ysv, y_[:], [(yk, 0), (yk, 1)], [("ys", ex, j)], yk)
            if ex + NW < NEXP:
                load_w(ex + NW)
        epipe.push(stage_b)
    epipe.flush()
    S.barrier(dummy)
    if STOP == "moe":
        S.emit()
        return nc

    for gt in range(NTOK // 128):
        sl = gt % 2
        dma("sp", cb0[sl][:], X1S[gt * 128:(gt + 1) * 128, :], [], [("cb0", sl)], ("cb0", sl))
        gather(cy0[sl][:], YS, dest[:, gt, 0:1], [], [("cy0", sl)], ("cy0", sl), NROWS)
        gather(cy1[sl][:], YS, dest[:, gt, 1:2], [], [("cy1", sl)], ("cy1", sl), NROWS)
        stt("dve", cb0[sl][:], cy0[sl][:], wgt[:, gt, 0:1], cb0[sl][:], ALU.mult, ALU.add,
            [("cb0", sl), ("cy0", sl)], [("cb0", sl)])
        stt("dve", cb0[sl][:], cy1[sl][:], wgt[:, gt, 1:2], cb0[sl][:], ALU.mult, ALU.add,
            [("cb0", sl), ("cy1", sl)], [("cb0", sl)])
        dma("sp", OUT[gt * 128:(gt + 1) * 128, :], cb0[sl][:], [("cb0", sl)], [("out", gt)], ("outst", sl))
    S.emit()
    return nc


_NC_CACHE = {}


def kernel(**inputs):
    if "nc" not in _NC_CACHE:
        _NC_CACHE["nc"] = build_nc()
    nc = _NC_CACHE["nc"]
    in_maps = []
    for c in range(NCORES):
        m = {}
        for k, v in inputs.items():
            v = np.asarray(v)
            if k in ("x", "mem", "positions"):
                m[k] = np.ascontiguousarray(v[NSEQ * c:NSEQ * (c + 1)])
            else:
                m[k] = np.ascontiguousarray(v)
        in_maps.append(m)
    res = run_bass_kernel_spmd(nc, in_maps, core_ids=list(range(NCORES)))
    kernel.last = res
    out = np.concatenate([np.asarray(r["out"]).reshape(NSEQ, SEQ, D) for r in res.results], axis=0)
    return out.astype(np.float32)
```
